# Optimizing a Trainium2 kernel written in Bass

```python
import math
import jax, jax.numpy as jnp
from jax import lax
import numpy as np

D_MODEL = 1024
BATCH = 4
SEQ = 8192
DEPTH = 2

GRID_W = 64
CTX_LEN = 256
HEAD_DIM = 64
A_HEADS = 4
A_KV_HEADS = 2
B_HEADS = 4
B_DK = 32
B_DV = 64
C_HEADS = 4
C_KV_HEADS = 2
WINDOW = 128
D_HEADS = 4
NA_ROWS_MAX = 8
NA_COLS = 16
Q_BLOCK = 128
N_BAND = 1 + 2 * (WINDOW // Q_BLOCK)
ROPE_THETA = 10000.0
N_BRANCH = 4
BRANCH_W = 256
N_EXPERTS = 32
TOP_K = 4
D_FF = D_MODEL
SWIGLU_LIMIT = 7.0
SWIGLU_ALPHA = 1.702
EXPERT_BLOCK = 256
N_MOD = 6
EPS = 1e-6
NEG_INF = -1e30
Q_SIZES = (A_HEADS * HEAD_DIM, B_HEADS * 2 * B_DK, C_HEADS * HEAD_DIM, D_HEADS * HEAD_DIM)
K_SIZES = (A_KV_HEADS * HEAD_DIM, B_HEADS * 2 * B_DK, C_KV_HEADS * HEAD_DIM, D_HEADS * HEAD_DIM)
V_SIZES = (A_KV_HEADS * HEAD_DIM, B_HEADS * B_DV, C_KV_HEADS * HEAD_DIM, D_HEADS * HEAD_DIM)
Q_TOT = sum(Q_SIZES)
KV_TOT = sum(K_SIZES) + sum(V_SIZES)
GATE_TOT = N_BRANCH * D_MODEL
W_IN_COLS = Q_TOT + KV_TOT + GATE_TOT

kernel_name = 'hybrid_diffusion_backbone'


def rms_norm(x, g):
    xf = x.astype(jnp.float32)
    y = xf * lax.rsqrt(jnp.mean(xf * xf, axis=-1, keepdims=True) + EPS)
    return (y * g.astype(jnp.float32)).astype(x.dtype)


def modulate(h, shift, scale):
    return h * (1 + scale) + shift


def heads(x, n):
    return x.reshape(x.shape[:-1] + (n, -1))


def split_cols(x, sizes):
    return jnp.split(x, np.cumsum(sizes)[:-1].tolist(), axis=-1)


def rope_2d(n_tok, dim):
    t = jnp.arange(n_tok, dtype=jnp.int32)
    row = (t // GRID_W).astype(jnp.float32)
    col = (t % GRID_W).astype(jnp.float32)
    n_freq = dim // 4
    inv = ROPE_THETA ** (-jnp.arange(n_freq, dtype=jnp.float32) / n_freq)
    ang = jnp.concatenate([row[:, None] * inv, col[:, None] * inv], axis=-1)
    return (jnp.cos(ang), jnp.sin(ang))


def apply_rope(x, cos, sin):
    half = x.shape[-1] // 2
    shape = (cos.shape[0],) + (1,) * (x.ndim - 3) + (half,)
    cos = cos.reshape(shape).astype(x.dtype)
    sin = sin.reshape(shape).astype(x.dtype)
    x1, x2 = x[..., :half], x[..., half:]
    return jnp.concatenate([x1 * cos - x2 * sin, x1 * sin + x2 * cos], axis=-1)


def softmax_sink(s, sink):
    m = jnp.maximum(jnp.max(s, axis=-1, keepdims=True), sink)
    e = jnp.exp(s - m)
    return e / (jnp.sum(e, axis=-1, keepdims=True) + jnp.exp(sink - m))


def sweep_query_blocks(attend, q):
    B, S = q.shape[:2]
    nb = S // Q_BLOCK
    qb = jnp.moveaxis(q.reshape((B, nb, Q_BLOCK) + q.shape[2:]), 1, 0)
    o = lax.map(attend, qb)
    return jnp.moveaxis(o, 0, 1).reshape((B, S) + o.shape[3:])


def gqa_attend(q, k, v, sink=None):
    B, Q, H, d = q.shape
    Hkv = k.shape[2]
    G = H // Hkv
    s = jnp.einsum('bqkgd,btkd->bkgqt', q.reshape(B, Q, Hkv, G, d), k).astype(jnp.float32) * (d ** -0.5)
    if sink is None:
        p = jax.nn.softmax(s, axis=-1)
    else:
        p = softmax_sink(s, sink.astype(jnp.float32).reshape(Hkv, G, 1, 1))
    o = jnp.einsum('bkgqt,btkd->bqkgd', p.astype(v.dtype), v)
    return o.reshape(B, Q, H, v.shape[-1])


def diff_lambda(lq1, lk1, lq2, lk2, lam_init):
    f = lambda a: a.astype(jnp.float32)
    return jnp.exp(jnp.sum(f(lq1) * f(lk1))) - jnp.exp(jnp.sum(f(lq2) * f(lk2))) + lam_init


def diff_attend(q, k, v, lam):
    s = jnp.einsum('bqhcd,bthcd->bchqt', q, k).astype(jnp.float32) * (q.shape[-1] ** -0.5)
    p = jax.nn.softmax(s, axis=-1)
    a = (p[:, 0] - lam * p[:, 1]).astype(v.dtype)
    return jnp.einsum('bhqt,bthd->bqhd', a, v)


def diff_out(o, subln_g, lam_init):
    return rms_norm(o, subln_g) * (1 - lam_init)


def window_attend(q, k, v, kc, vc, sink):
    B, S, H, d = q.shape
    Hkv = k.shape[2]
    G = H // Hkv
    nb = S // Q_BLOCK
    pad = ((0, 0), (WINDOW, WINDOW), (0, 0), (0, 0))
    idx = jnp.arange(nb)[:, None] + jnp.arange(N_BAND)[None, :]
    J = N_BAND * Q_BLOCK
    kb = jnp.pad(k, pad).reshape(B, nb + N_BAND - 1, Q_BLOCK, Hkv, d)[:, idx].reshape(B, nb, J, Hkv, d)
    vb = jnp.pad(v, pad).reshape(B, nb + N_BAND - 1, Q_BLOCK, Hkv, v.shape[-1])[:, idx].reshape(B, nb, J, Hkv, v.shape[-1])
    qb = q.reshape(B, nb, Q_BLOCK, Hkv, G, d)
    scale = d ** -0.5
    s_loc = jnp.einsum('bnqkgd,bnjkd->bnkgqj', qb, kb).astype(jnp.float32) * scale
    qpos = jnp.arange(nb)[:, None] * Q_BLOCK + jnp.arange(Q_BLOCK)[None, :]
    kpos = jnp.arange(nb)[:, None] * Q_BLOCK - WINDOW + jnp.arange(J)[None, :]
    valid = (jnp.abs(kpos[:, None, :] - qpos[:, :, None]) <= WINDOW) & (kpos[:, None, :] >= 0) & (kpos[:, None, :] < S)
    s_loc = jnp.where(valid[None, :, None, None], s_loc, NEG_INF)
    s_ctx = jnp.einsum('bnqkgd,bckd->bnkgqc', qb, kc).astype(jnp.float32) * scale
    p = softmax_sink(jnp.concatenate([s_loc, s_ctx], axis=-1), sink.astype(jnp.float32).reshape(1, 1, Hkv, G, 1, 1))
    p = p.astype(v.dtype)
    o = jnp.einsum('bnkgqj,bnjkd->bnqkgd', p[..., :J], vb) + jnp.einsum('bnkgqc,bckd->bnqkgd', p[..., J:], vc)
    return o.reshape(B, S, H, v.shape[-1])


def neighbourhood_attend(q, k, v, kc, vc, rpb):
    B, S, H, d = q.shape
    rows = S // GRID_W
    kh = min(NA_ROWS_MAX, rows)
    J = kh * GRID_W
    qg = q.reshape(B, rows, GRID_W, H, d)
    r = jnp.arange(rows)
    r0 = jnp.clip(r - kh // 2, 0, rows - kh)
    ridx = r0[:, None] + jnp.arange(kh)[None, :]
    kn = k.reshape(B, rows, GRID_W, H, d)[:, ridx].reshape(B, rows, J, H, d)
    vn = v.reshape(B, rows, GRID_W, H, v.shape[-1])[:, ridx].reshape(B, rows, J, H, v.shape[-1])
    scale = d ** -0.5
    s_loc = jnp.einsum('brqhd,brjhd->brhqj', qg, kn).astype(jnp.float32) * scale
    col = jnp.arange(GRID_W)
    c0 = jnp.clip(col - NA_COLS // 2, 0, GRID_W - NA_COLS)
    col_ok = (col[None, :] >= c0[:, None]) & (col[None, :] < c0[:, None] + NA_COLS)
    mask = jnp.broadcast_to(col_ok[:, None, :], (GRID_W, kh, GRID_W)).reshape(GRID_W, J)
    dr = ridx - r[:, None] + (NA_ROWS_MAX - 1)
    dc = jnp.clip(col[None, :] - col[:, None], -(NA_COLS - 1), NA_COLS - 1) + (NA_COLS - 1)
    bias = rpb[:, dr[:, None, :, None], dc[None, :, None, :]]
    bias = bias.transpose(1, 0, 2, 3, 4).reshape(rows, H, GRID_W, J).astype(jnp.float32)
    s_loc = jnp.where(mask, s_loc + bias, NEG_INF)
    s_ctx = jnp.einsum('brqhd,bchd->brhqc', qg, kc).astype(jnp.float32) * scale
    p = jax.nn.softmax(jnp.concatenate([s_loc, s_ctx], axis=-1), axis=-1).astype(v.dtype)
    o = jnp.einsum('brhqj,brjhd->brqhd', p[..., :J], vn) + jnp.einsum('brhqc,bchd->brqhd', p[..., J:], vc)
    return o.reshape(B, S, H, v.shape[-1])


def merge_branches(outs, g, b_gate, w_branch, w_out):
    lead = outs[0].shape[:2]
    br = jnp.stack([o.reshape(lead + (BRANCH_W,)) for o in outs], axis=2)
    proj = jnp.einsum('btnc,ncd->btnd', br, w_branch)
    gate = jax.nn.sigmoid(g + b_gate).reshape(lead + (N_BRANCH, D_MODEL))
    return jnp.sum(gate * proj, axis=2) @ w_out


def token_mixer(h, hc, need_ctx, lam_init, rope, w_in, b_gate, a_qn, a_kn, b_qn, b_kn,
                lam_q1, lam_k1, lam_q2, lam_k2, subln_g, c_qn, c_kn, sink, d_qn, d_kn,
                rpb, w_branch, w_out):
    cos64, sin64, cos32, sin32 = rope
    B, S, _ = h.shape
    C = hc.shape[1]
    q, kv, g = split_cols(h @ w_in, (Q_TOT, KV_TOT, GATE_TOT))
    q_g, q_d, q_w, q_n = split_cols(q, Q_SIZES)
    k_g, k_d, k_w, k_n, v_g, v_d, v_w, v_n = split_cols(kv, K_SIZES + V_SIZES)
    ck_g, ck_d, ck_w, ck_n, cv_g, cv_d, cv_w, cv_n = split_cols(hc @ w_in[:, Q_TOT:Q_TOT + KV_TOT], K_SIZES + V_SIZES)

    q_g = apply_rope(rms_norm(heads(q_g, A_HEADS), a_qn), cos64, sin64)
    k_g = apply_rope(rms_norm(heads(k_g, A_KV_HEADS), a_kn), cos64, sin64)
    ck_g = rms_norm(heads(ck_g, A_KV_HEADS), a_kn)
    cv_g = heads(cv_g, A_KV_HEADS)
    kk_g = jnp.concatenate([k_g, ck_g], axis=1)
    vv_g = jnp.concatenate([heads(v_g, A_KV_HEADS), cv_g], axis=1)
    o_g = sweep_query_blocks(lambda qb: gqa_attend(qb, kk_g, vv_g), q_g)

    lam = diff_lambda(lam_q1, lam_k1, lam_q2, lam_k2, lam_init)
    q_d = apply_rope(rms_norm(q_d.reshape(B, S, B_HEADS, 2, B_DK), b_qn), cos32, sin32)
    k_d = apply_rope(rms_norm(k_d.reshape(B, S, B_HEADS, 2, B_DK), b_kn), cos32, sin32)
    ck_d = rms_norm(ck_d.reshape(B, C, B_HEADS, 2, B_DK), b_kn)
    cv_d = heads(cv_d, B_HEADS)
    kk_d = jnp.concatenate([k_d, ck_d], axis=1)
    vv_d = jnp.concatenate([heads(v_d, B_HEADS), cv_d], axis=1)
    o_d = diff_out(sweep_query_blocks(lambda qb: diff_attend(qb, kk_d, vv_d, lam), q_d), subln_g, lam_init)

    q_w = apply_rope(rms_norm(heads(q_w, C_HEADS), c_qn), cos64, sin64)
    k_w = apply_rope(rms_norm(heads(k_w, C_KV_HEADS), c_kn), cos64, sin64)
    ck_w = rms_norm(heads(ck_w, C_KV_HEADS), c_kn)
    cv_w = heads(cv_w, C_KV_HEADS)
    o_w = window_attend(q_w, k_w, heads(v_w, C_KV_HEADS), ck_w, cv_w, sink)

    q_n = rms_norm(heads(q_n, D_HEADS), d_qn)
    k_n = rms_norm(heads(k_n, D_HEADS), d_kn)
    ck_n = rms_norm(heads(ck_n, D_HEADS), d_kn)
    cv_n = heads(cv_n, D_HEADS)
    o_n = neighbourhood_attend(q_n, k_n, heads(v_n, D_HEADS), ck_n, cv_n, rpb)

    out = merge_branches((o_g, o_d, o_w, o_n), g, b_gate, w_branch, w_out)
    if not need_ctx:
        return out, None
    cq_g, cq_d, cq_w, cq_n = split_cols(hc @ w_in[:, :Q_TOT], Q_SIZES)
    co_g = gqa_attend(rms_norm(heads(cq_g, A_HEADS), a_qn), ck_g, cv_g)
    co_d = diff_out(diff_attend(rms_norm(cq_d.reshape(B, C, B_HEADS, 2, B_DK), b_qn), ck_d, cv_d, lam), subln_g, lam_init)
    co_w = gqa_attend(rms_norm(heads(cq_w, C_HEADS), c_qn), ck_w, cv_w, sink)
    co_n = gqa_attend(rms_norm(heads(cq_n, D_HEADS), d_qn), ck_n, cv_n)
    out_c = merge_branches((co_g, co_d, co_w, co_n), hc @ w_in[:, Q_TOT + KV_TOT:], b_gate, w_branch, w_out)
    return out, out_c


def moe_ffn(h, router_w, router_b, w_gate_up, b_gate_up, w_down, b_down):
    T, D = h.shape
    logits = (h @ router_w).astype(jnp.float32) + router_b.astype(jnp.float32)
    top_v, top_e = lax.top_k(logits, TOP_K)
    top_w = jax.nn.softmax(top_v, axis=-1).astype(h.dtype)
    n = T * TOP_K
    flat_e = top_e.reshape(-1)
    order = jnp.argsort(flat_e, stable=True)
    e_sorted = flat_e[order]
    tok = (order // TOP_K).astype(jnp.int32)
    counts = jnp.bincount(flat_e, length=N_EXPERTS)
    padded = (counts + EXPERT_BLOCK - 1) // EXPERT_BLOCK * EXPERT_BLOCK
    pad_end = jnp.cumsum(padded)
    pad_start = pad_end - padded
    start = jnp.cumsum(counts) - counts
    dest = pad_start[e_sorted] + jnp.arange(n) - start[e_sorted]
    P = n + N_EXPERTS * EXPERT_BLOCK
    n_blk = P // EXPERT_BLOCK
    slot_tok = jnp.full((P,), T, jnp.int32).at[dest].set(tok)
    hp = jnp.concatenate([h, jnp.zeros((1, D), h.dtype)], axis=0)
    xbuf = hp[slot_tok].reshape(n_blk, EXPERT_BLOCK, D)
    blk_e = jnp.minimum(jnp.searchsorted(pad_end, jnp.arange(n_blk) * EXPERT_BLOCK, side='right'), N_EXPERTS - 1)

    def expert_block(args):
        xb, e = args
        gu = xb @ w_gate_up[e] + b_gate_up[e]
        gate = jnp.minimum(gu[:, :D_FF], SWIGLU_LIMIT)
        up = jnp.clip(gu[:, D_FF:], -SWIGLU_LIMIT, SWIGLU_LIMIT)
        return (gate * jax.nn.sigmoid(SWIGLU_ALPHA * gate) * (up + 1)) @ w_down[e] + b_down[e]

    y = lax.map(expert_block, (xbuf, blk_e)).reshape(P, D)
    contrib = y[dest] * top_w.reshape(-1)[order][:, None]
    return jax.ops.segment_sum(contrib, tok, num_segments=T)


def _normal(k, shape, scale):
    return jax.random.normal(k, shape, jnp.float32) * scale


def _gain(k, shape):
    return 1.0 + 0.05 * jax.random.normal(k, shape, jnp.float32)


def setup_inputs(seed: int = 0) -> dict:
    key = jax.random.key(seed)
    ks = jax.random.split(key, 33)
    L, D, E = DEPTH, D_MODEL, N_EXPERTS
    return {
        'x': _normal(ks[0], (BATCH, SEQ, D), 1.0),
        'c': _normal(ks[1], (BATCH, D), 1.0),
        'ctx': _normal(ks[2], (BATCH, CTX_LEN, D), 1.0),
        'c_ctx': _normal(ks[3], (D,), 1.0),
        'norm1_g': _gain(ks[4], (L, D)),
        'norm2_g': _gain(ks[5], (L, D)),
        'w_ada': _normal(ks[6], (L, D, N_MOD * D), 0.5 * D ** -0.5),
        'b_ada': _normal(ks[7], (L, N_MOD * D), 0.1),
        'w_in': _normal(ks[8], (L, D, W_IN_COLS), D ** -0.5),
        'b_gate': _normal(ks[9], (L, GATE_TOT), 0.1),
        'a_qn': _gain(ks[10], (L, HEAD_DIM)),
        'a_kn': _gain(ks[11], (L, HEAD_DIM)),
        'b_qn': _gain(ks[12], (L, B_DK)),
        'b_kn': _gain(ks[13], (L, B_DK)),
        'lam_q1': _normal(ks[14], (L, B_DK), 0.1),
        'lam_k1': _normal(ks[15], (L, B_DK), 0.1),
        'lam_q2': _normal(ks[16], (L, B_DK), 0.1),
        'lam_k2': _normal(ks[17], (L, B_DK), 0.1),
        'subln_g': _gain(ks[18], (L, B_DV)),
        'c_qn': _gain(ks[19], (L, HEAD_DIM)),
        'c_kn': _gain(ks[20], (L, HEAD_DIM)),
        'sink': _normal(ks[21], (L, C_HEADS), 0.5),
        'd_qn': _gain(ks[22], (L, HEAD_DIM)),
        'd_kn': _gain(ks[23], (L, HEAD_DIM)),
        'rpb': _normal(ks[24], (L, D_HEADS, 2 * NA_ROWS_MAX - 1, 2 * NA_COLS - 1), 0.1),
        'w_branch': _normal(ks[25], (L, N_BRANCH, BRANCH_W, D), BRANCH_W ** -0.5),
        'w_out': _normal(ks[26], (L, D, D), D ** -0.5),
        'router_w': _normal(ks[27], (L, D, E), D ** -0.5),
        'router_b': _normal(ks[28], (L, E), 0.01),
        'w_gate_up': _normal(ks[29], (L, E, D, 2 * D_FF), D ** -0.5),
        'b_gate_up': _normal(ks[30], (L, E, 2 * D_FF), 0.02),
        'w_down': _normal(ks[31], (L, E, D_FF, D), D_FF ** -0.5),
        'b_down': _normal(ks[32], (L, E, D), 0.02),
    }


def reference(x, c, ctx, c_ctx, norm1_g, norm2_g, w_ada, b_ada, w_in, b_gate,
              a_qn, a_kn, b_qn, b_kn, lam_q1, lam_k1, lam_q2, lam_k2, subln_g,
              c_qn, c_kn, sink, d_qn, d_kn, rpb, w_branch, w_out,
              router_w, router_b, w_gate_up, b_gate_up, w_down, b_down):
    B, S, D = x.shape
    rope = rope_2d(S, HEAD_DIM) + rope_2d(S, B_DK)
    for l in range(DEPTH):
        last = l == DEPTH - 1
        lam_init = 0.8 - 0.6 * math.exp(-0.3 * l)
        mod = (jax.nn.silu(c) @ w_ada[l] + b_ada[l])[:, None, :]
        sh1, sc1, g1, sh2, sc2, g2 = jnp.split(mod, N_MOD, axis=-1)
        cmod = jax.nn.silu(c_ctx) @ w_ada[l] + b_ada[l]
        csh1, csc1, cg1, csh2, csc2, cg2 = jnp.split(cmod, N_MOD, axis=-1)
        h = modulate(rms_norm(x, norm1_g[l]), sh1, sc1)
        hc = modulate(rms_norm(ctx, norm1_g[l]), csh1, csc1)
        mix, mix_c = token_mixer(h, hc, not last, lam_init, rope, w_in[l], b_gate[l], a_qn[l], a_kn[l],
                                 b_qn[l], b_kn[l], lam_q1[l], lam_k1[l], lam_q2[l], lam_k2[l], subln_g[l],
                                 c_qn[l], c_kn[l], sink[l], d_qn[l], d_kn[l], rpb[l], w_branch[l], w_out[l])
        x = x + g1 * mix
        h = modulate(rms_norm(x, norm2_g[l]), sh2, sc2)
        expert_args = (router_w[l], router_b[l], w_gate_up[l], b_gate_up[l], w_down[l], b_down[l])
        if last:
            x = x + g2 * moe_ffn(h.reshape(-1, D), *expert_args).reshape(B, S, D)
        else:
            ctx = ctx + cg1 * mix_c
            hc = modulate(rms_norm(ctx, norm2_g[l]), csh2, csc2)
            y = moe_ffn(jnp.concatenate([h.reshape(-1, D), hc.reshape(-1, D)], axis=0), *expert_args)
            x = x + g2 * y[:B * S].reshape(B, S, D)
            ctx = ctx + cg2 * y[B * S:].reshape(ctx.shape)
    return x
```

```python
import contextlib
import math
import numpy as np
import ml_dtypes
import concourse.bass as bass
import concourse.mybir as mybir
from concourse.bass_utils import run_bass_kernel_spmd

F32 = mybir.dt.float32
BF16 = mybir.dt.bfloat16
AF = mybir.ActivationFunctionType
ALU = mybir.AluOpType
AX = mybir.AxisListType

D = 1024
SEQ = 8192
HALF = 4096
CTX = 256
NT = 34
TOK = NT * 128
NE = 32
EPS = 1e-6
GRID_W = 64
TBS = [(i * 512, 512) for i in range(8)] + [(4096, 256)]
NPAT = 27


class Sem:
    def __init__(self, sem, name):
        self.sem = sem
        self.cnt = 0
        self.name = name


class Eng:
    def __init__(self, name, eng, sem):
        self.name = name
        self.eng = eng
        self.s = sem
        self.waited = {}


class Buf:
    __slots__ = ("w", "r", "name")

    def __init__(self, name=""):
        self.w = None
        self.r = {}
        self.name = name


class Builder:
    def __init__(self, nc, st):
        self.nc = nc
        self.st = st
        mk = lambda n: Sem(st.enter_context(nc.semaphore(n)), n)
        self.pe = Eng("pe", nc.tensor, mk("s_pe"))
        self.act = Eng("act", nc.scalar, mk("s_act"))
        self.dve = Eng("dve", nc.vector, mk("s_dve"))
        self.pool = Eng("pool", nc.gpsimd, mk("s_pool"))
        self.sp = Eng("sp", nc.sync, mk("s_sp"))
        self.engs = [self.pe, self.act, self.dve, self.pool, self.sp]
        self.dsems_hw = [mk("s_d%d" % i) for i in range(16)]
        self.dsems_sw = [mk("s_w%d" % i) for i in range(8)]
        self.dsems = self.dsems_hw + self.dsems_sw
        self.dnext = {"hw": 0, "sw": 0}
        self.csem = mk("s_cc")
        self.uid = 0

    def _wait(self, E, S, v):
        if v <= 0:
            return
        if E.waited.get(id(S), 0) >= v:
            return
        E.eng.wait_ge(S.sem, v)
        E.waited[id(S)] = v

    def _deps(self, E, reads, writes, is_dma):
        for b in reads:
            if b.w is not None:
                S, v = b.w
                self._wait(E, S, v)
        for b in writes:
            if b.w is not None:
                S, v = b.w
                if is_dma or S is not E.s or E.name != "pe":
                    self._wait(E, S, v)
            for S, v in b.r.values():
                self._wait(E, S, v)

    def _mark(self, tok, reads, writes):
        S = tok[0]
        for b in reads:
            b.r[id(S)] = tok
        for b in writes:
            b.w = tok
            b.r = {}

    def op(self, E, fn, reads=(), writes=()):
        self._deps(E, reads, writes, False)
        ins = fn(E.eng)
        E.s.cnt += 1
        ins.then_inc(E.s.sem, 1)
        self._mark((E.s, E.s.cnt), reads, writes)
        return ins

    def dma(self, E, out, in_, reads=(), writes=()):
        self._deps(E, reads, writes, True)
        kind = "sw" if E.name == "pool" else "hw"
        pool = self.dsems_sw if kind == "sw" else self.dsems_hw
        d = pool[self.dnext[kind]]
        self.dnext[kind] = (self.dnext[kind] + 1) % len(pool)
        self._wait(E, d, d.cnt)
        ins = E.eng.dma_start(out=out, in_=in_)
        d.cnt += 16
        ins.then_inc(d.sem, 16)
        self._mark((d, d.cnt), reads, writes)

    def coll(self, kind, ins, outs, groups):
        ins_ = self.pool.eng.collective_compute(kind, ALU.bypass, replica_groups=groups, ins=ins, outs=outs)
        self.csem.cnt += 1
        ins_.then_inc(self.csem.sem, 1)

    def barrier(self):
        sems = [e.s for e in self.engs] + self.dsems + [self.csem]
        for E in self.engs:
            for S in sems:
                self._wait(E, S, S.cnt)

    def final_wait(self):
        for S in self.dsems:
            self._wait(self.sp, S, S.cnt)
        for e in self.engs:
            self._wait(self.sp, e.s, e.s.cnt)

    def sb(self, st, shape, dt, name=None):
        self.uid += 1
        t = st.enter_context(self.nc.sbuf_tensor("%s_%d" % (name or "t", self.uid), list(shape), dt))
        return t, Buf(name or "t")

    def ring(self, st, n, shape, dt, name="r"):
        return Ring([self.sb(st, shape, dt, name + str(i)) for i in range(n)])


class Ring:
    def __init__(self, items):
        self.items = items
        self.i = 0

    def next(self):
        it = self.items[self.i]
        self.i = (self.i + 1) % len(self.items)
        return it


TOKB = 8448
NTB = 66
NPD = 27
TOKL = 4352
NTL = 34


def half_tiles(s, with_ctx=True):
    t = list(range(32))
    if with_ctx:
        t += [32, 33]
    return t


def half_blocks(s, with_ctx=True):
    b = [(i * 512, 512) for i in range(8)]
    if with_ctx:
        b.append((HALF, 256))
    return b


def loc(s, t0):
    return t0


def lslot(idx):
    if 0 <= idx <= 31:
        return idx
    if idx >= 32:
        assert idx - 32 < 2
        return idx
    assert idx >= -2
    return 36 + idx


def l_offsets(it, mixer):
    if mixer == "C":
        return [-1, 0, 1]
    if it == 0:
        return [-2, -1, 0, 1, 2, 3]
    if it == 31:
        return [-3, -2, -1, 0, 1, 2]
    return [-2, -1, 0, 1, 2]


def l_pid(it, o, mixer):
    if mixer == "C":
        if it == 0 and o == -1:
            return 3
        if it == 31 and o == 1:
            return 4
        return o + 1
    if it == 0:
        return 5 + (o + 2)
    if it == 1:
        return 11 + (o + 2)
    if it == 30:
        return 16 + (o + 2)
    if it == 31:
        return 21 + (o + 3)
    return o + 2


def d_keytiles(g):
    if g <= 1:
        return [0, 1, 2, 3]
    if g >= 62:
        return [60, 61, 62, 63]
    return [g - 2, g - 1, g, g + 1, g + 2]


def d_pid(g, kt):
    if g == 0:
        return 5 + kt
    if g == 1:
        return 9 + kt
    if g == 62:
        return 13 + (kt - 60)
    if g == 63:
        return 17 + (kt - 60)
    return kt - g + 2


class Prog:
    def __init__(self, layers=(0, 1), debug=None, limit=None, n_cores=8):
        self.n_cores = n_cores
        self.layers = layers
        self.debug = debug or {}
        self.limit = limit or {}
        self.nc = bass.Bass("TRN2", target_bir_lowering=False)
        self.ins = {}
        self.outs = {}

    def din(self, name, shape, dt=F32):
        t = self.nc.dram_tensor(name, list(shape), dt, kind="ExternalInput").ap()
        self.ins[name] = (tuple(shape), dt)
        return t

    def dscr(self, name, shape, dt, out=False):
        kind = "ExternalOutput" if (out or name in self.debug) else "Internal"
        t = self.nc.dram_tensor(name, list(shape), dt, kind=kind).ap()
        if kind == "ExternalOutput":
            self.outs[name] = (tuple(shape), dt)
        return t

    def build(self):
        nc = self.nc
        with contextlib.ExitStack() as st, nc.allow_low_precision("bf16 matmul per problem tolerance"):
            B = self.B = Builder(nc, st)
            self.gst = st
            self.xin = self.din("xin", [TOKL, D])
            self.cvec = self.din("cvec", [128, 16])
            self.consts_in()
            self.L = [self.layer_inputs(l) for l in range(2)]
            self.xcur = self.dscr("xcur", [TOKL, D], F32)
            self.qT_d = self.dscr("qT_d", [D, TOKL], BF16)
            self.ksend = [[self.dscr("ksend%d_%d" % (i, j), [256, HALF], BF16) for j in range(3)] for i in range(2)]
            self.vsend = [[self.dscr("vsend%d_%d" % (i, j), [1024, 768], BF16) for j in range(4)] for i in range(2)]
            self.kall = [[self.dscr("kall%d_%d" % (i, j), [512, HALF], BF16) for j in range(3)] for i in range(2)]
            self.vall = [[self.dscr("vall%d_%d" % (i, j), [2048, 768], BF16) for j in range(4)] for i in range(2)]
            self.kctx = self.dscr("kctx", [768, CTX], BF16)
            self.vctx = self.dscr("vctx", [CTX, 768], BF16)
            self.gT_d = self.dscr("gT_d", [4096, TOKL], BF16)
            self.br_d = self.dscr("br_d", [TOKL, D], BF16)
            self.hT_d = self.dscr("hT_d", [D, TOKL], BF16)
            self.y = self.dscr("y", [HALF, D], F32, out=True)
            self.banks = []
            for i in range(8):
                t = st.enter_context(nc.psum_tensor("ps%d" % i, [128, 512], F32))
                self.banks.append((t, Buf("ps%d" % i)))
            self.pring = Ring(self.banks)
            self.consts_sb()
            for l in self.layers:
                self.layer(l)
            B.barrier()
            B.final_wait()
        return nc

    def consts_in(self):
        self.c_ident = self.din("ident", [128, 128])
        self.c_blk64 = self.din("blk64", [128, 128])
        self.c_blk32 = self.din("blk32", [128, 128])
        self.c_rot64 = self.din("rot64", [128, 128])
        self.c_rot32 = self.din("rot32", [128, 128])
        self.c_cos64 = self.din("cos64", [128, HALF])
        self.c_sin64 = self.din("sin64", [128, HALF])
        self.c_cos32 = self.din("cos32", [128, HALF])
        self.c_sin32 = self.din("sin32", [128, HALF])
        self.c_sel = self.din("sel", [128, 2])
        self.c_cmask = self.din("cmask", [128, 5 * 128])
        self.c_dmask = self.din("dmask", [128, NPD * 128])

    def layer_inputs(self, l):
        s = "_%d" % l
        L = {}
        L["wada"] = self.din("wada" + s, [D, 6 * D])
        L["bada"] = self.din("bada" + s, [128, 48])
        L["badarow"] = self.din("badarow" + s, [1, 6 * D])
        L["n1g"] = self.din("n1g" + s, [128, 8])
        L["n2g"] = self.din("n2g" + s, [128, 8])
        L["win"] = self.din("win" + s, [D, 6656])
        L["bgate"] = self.din("bgate" + s, [128, 32])
        L["qkg"] = self.din("qkg" + s, [128, 14])
        L["lamv"] = self.din("lamv" + s, [128, 128])
        L["subg"] = self.din("subg" + s, [128, 64])
        L["sinkb"] = self.din("sinkb" + s, [128, 4])
        L["dtab"] = self.din("dtab" + s, [128, NPD * 4 * 128])
        L["wbr"] = self.din("wbr" + s, [D, D])
        L["wout"] = self.din("wout" + s, [D, D])
        L["rtw"] = self.din("rtw" + s, [D, NE])
        L["rtb"] = self.din("rtb" + s, [128, NE])
        L["wgu"] = self.din("wgu" + s, [NE * D, 2 * D])
        L["bgu"] = self.din("bgu" + s, [128, NE * 16])
        L["wdn"] = self.din("wdn" + s, [NE * D, D])
        L["bdn"] = self.din("bdn" + s, [NE, D])
        return L

    def consts_sb(self):
        B, st = self.B, self.gst
        self.ident, self.ident_b = B.sb(st, [128, 128], F32, "ident")
        B.dma(B.sp, self.ident[:], self.c_ident[:, :], writes=[self.ident_b])
        self.identb, self.identb_b = B.sb(st, [128, 128], BF16, "identb")
        B.dma(B.pool, self.identb[:], self.c_ident[:, :], writes=[self.identb_b])
        self.cb = {}
        for nm, src in (("blk64", self.c_blk64), ("blk32", self.c_blk32), ("rot64", self.c_rot64), ("rot32", self.c_rot32)):
            t, b = B.sb(st, [128, 128], BF16, nm)
            B.dma(B.pool, t[:], src[:, :], writes=[b])
            self.cb[nm] = (t, b)
        self.ones1, self.ones1_b = B.sb(st, [1, 128], F32, "ones1")
        B.op(B.dve, lambda e: e.memset(self.ones1[:], 1.0), writes=[self.ones1_b])

    def layer(self, l):
        self.l = l
        self.Lw = self.L[l]
        self.need_ctx = (l == 0)
        B = self.B
        stop = self.limit.get("stop")
        with contextlib.ExitStack() as lst:
            self.lst = lst
            self.phase_mod()
            for s in (0,):
                with contextlib.ExitStack() as pst:
                    self.hT, self.hT_b = B.sb(pst, [128, 8, 4352], BF16, "hT")
                    self.phase_norm(1, s)
                    self.phase_proj(s)
            self.phase_gather()
            if stop == "proj":
                return
            with contextlib.ExitStack() as ast_:
                self.ast = ast_
                self.phase_attn_setup()
                for s in (0,):
                    for mixer in self.limit.get("mixers", "ABCD"):
                        if mixer in "AB":
                            self.attn_global(s, mixer)
                        else:
                            self.attn_local(s, mixer)
                    if stop == "attn":
                        continue
                    self.phase_merge(s)
                    if stop == "merge":
                        continue
                    self.phase_norm(2, s)
            if stop in ("attn", "merge", "norm2"):
                return
            self.phase_moe()

    def phase_mod(self):
        B, nc, Lw = self.B, self.nc, self.Lw
        lst = self.lst
        self.modT, self.modT_b = B.sb(lst, [128, 48, 2], F32, "modT")
        self.G, self.G_b = B.sb(lst, [128, 2, 2, 8], F32, "G")
        self.rw, self.rw_b = B.sb(lst, [128, NTL, NE], F32, "rw")
        self.gbc = {}
        for w in (1, 2):
            for j in (0, 1):
                self.gbc[(w, j)] = B.sb(lst, [128, D], F32, "gbc%d%d" % (w, j))
        with contextlib.ExitStack() as st:
            cv, cv_b = B.sb(st, [128, 16], F32, "cv")
            sl, sl_b = B.sb(st, [128, 16], F32, "silu")
            rep, rep_b = B.sb(st, [128, 16, 128], F32, "rep")
            bada, bada_b = B.sb(st, [128, 48], F32, "bada")
            brow, brow_b = B.sb(st, [1, 6 * D], F32, "brow")
            n1g, n1g_b = B.sb(st, [128, 8], F32, "n1g")
            n2g, n2g_b = B.sb(st, [128, 8], F32, "n2g")
            tmp, tmp_b = B.sb(st, [128, 8], F32, "tmpm")
            war = B.ring(st, 2, [128, 8, 512], F32, "wa")
            B.dma(B.sp, cv[:], self.cvec[:, :], writes=[cv_b])
            B.dma(B.sp, bada[:], Lw["bada"][:, :], writes=[bada_b])
            B.dma(B.sp, brow[:], Lw["badarow"][:, :], writes=[brow_b])
            B.dma(B.sp, n1g[:], Lw["n1g"][:, :], writes=[n1g_b])
            B.dma(B.sp, n2g[:], Lw["n2g"][:, :], writes=[n2g_b])
            B.op(B.act, lambda e: e.activation(out=sl[:], in_=cv[:], func=AF.Silu), reads=[cv_b], writes=[sl_b])
            for i in range(16):
                B.op(B.dve, lambda e: e.tensor_copy(out=rep[:, i, :], in_=sl[:, i:i + 1].to_broadcast([128, 128])),
                     reads=[sl_b], writes=[rep_b])
            wsrc = Lw["wada"].rearrange("(k p) n -> p k n", p=128)
            pm, pm_b = self.pring.next()
            for c in range(12):
                wa, wa_b = war.next()
                B.dma(B.sp, wa[:], wsrc[:, :, c * 512:(c + 1) * 512], writes=[wa_b])
                for i in range(4):
                    cc = c * 4 + i
                    for k in range(8):
                        B.op(B.pe, lambda e: e.matmul(pm[:, 2 * cc:2 * cc + 2], lhsT=wa[:, k, i * 128:(i + 1) * 128],
                                                      rhs=sl[:, 2 * k:2 * k + 2], start=(k == 0), stop=(k == 7),
                                                      skip_group_check=True),
                             reads=[wa_b, sl_b], writes=[pm_b])
                if c in (4, 5, 10, 11):
                    w = 1 if c < 6 else 2
                    half = c % 2
                    for j in (0, 1):
                        pb, pb_b = self.pring.next()
                        if pb_b is pm_b:
                            pb, pb_b = self.pring.next()
                        for k in range(8):
                            B.op(B.pe, lambda e: e.matmul(pb[:, :], lhsT=rep[:, 2 * k + j, :], rhs=wa[:, k, :],
                                                          start=(k == 0), stop=False, skip_group_check=True),
                                 reads=[rep_b, wa_b], writes=[pb_b])
                        B.op(B.pe, lambda e: e.matmul(pb[:, :], lhsT=self.ones1[:, :], rhs=brow[0:1, c * 512:(c + 1) * 512],
                                                      start=False, stop=True, skip_group_check=True),
                             reads=[self.ones1_b, brow_b], writes=[pb_b])
                        gt, gb = self.gbc[(w, j)]
                        B.op(B.act, lambda e: e.activation(out=gt[:, half * 512:(half + 1) * 512], in_=pb[:, :], func=AF.Copy),
                             reads=[pb_b], writes=[gb])
            B.op(B.dve, lambda e: e.tensor_tensor(out=self.modT[:], in0=pm[:, 0:96].rearrange("p (c j) -> p c j", j=2),
                                                  in1=bada[:].unsqueeze(2).to_broadcast([128, 48, 2]), op=ALU.add),
                 reads=[pm_b, bada_b], writes=[self.modT_b])
            for w, (ng, ng_b, sc0) in ((0, (n1g, n1g_b, 8)), (1, (n2g, n2g_b, 32))):
                for j in (0, 1):
                    B.op(B.dve, lambda e: e.tensor_scalar(out=tmp[:], in0=self.modT[:, sc0:sc0 + 8, j], scalar1=1.0,
                                                          scalar2=None, op0=ALU.add),
                         reads=[self.modT_b], writes=[tmp_b])
                    B.op(B.dve, lambda e: e.tensor_tensor(out=self.G[:, w, j, :], in0=tmp[:], in1=ng[:], op=ALU.mult),
                         reads=[tmp_b, ng_b], writes=[self.G_b])
            B.barrier()

    def phase_norm(self, which, s):
        B, nc = self.B, self.nc
        l = self.l
        w = which - 1
        sh0 = 0 if which == 1 else 24
        src = self.xin if (l == 0 and which == 1) else self.xcur
        tiles = half_tiles(s, with_ctx=True)
        with contextlib.ExitStack() as st:
            xr = B.ring(st, 3, [128, D], F32, "xt")
            sqr = B.ring(st, 2, [128, D], F32, "sq")
            xsr = B.ring(st, 4, [128, D], F32, "xs")
            ssr = B.ring(st, 4, [128, 4], F32, "ss")
            tmr = B.ring(st, 3, [128, 8, 128], F32, "tm")
            hfr = B.ring(st, 4, [128, 8, 128], F32, "hf")
            hbr = B.ring(st, 3, [128, 8, 128], BF16, "hb")
            if which == 2:
                self.router_setup(st)
            if which == 2 and "moe_tiles" in self.limit:
                tiles = [t for t in tiles if t in self.limit["moe_tiles"]]
            tiles = [r for r in tiles if not (which == 2 and r >= 32 and not self.need_ctx)]

            def stage1(r):
                xt, xt_b = xr.next()
                sq, sq_b = sqr.next()
                xs, xs_b = xsr.next()
                ss, ss_b = ssr.next()
                B.dma(B.sp, xt[:], src[r * 128:(r + 1) * 128, :], writes=[xt_b])
                B.op(B.dve, lambda e: e.tensor_tensor(out=sq[:], in0=xt[:], in1=xt[:], op=ALU.mult), reads=[xt_b], writes=[sq_b])
                B.op(B.dve, lambda e: e.reduce_sum(out=ss[:, 0:1], in_=sq[:], axis=AX.X), reads=[sq_b], writes=[ss_b])
                B.op(B.act, lambda e: e.activation(out=ss[:, 1:2], in_=ss[:, 0:1], func=AF.Sqrt, bias=EPS, scale=1.0 / D),
                     reads=[ss_b], writes=[ss_b])
                B.op(B.dve, lambda e: e.reciprocal(out=ss[:, 2:3], in_=ss[:, 1:2]), reads=[ss_b], writes=[ss_b])
                B.op(B.act, lambda e: e.activation(out=xs[:], in_=xt[:], func=AF.Copy, scale=ss[:, 2:3]),
                     reads=[xt_b, ss_b], writes=[xs_b])
                return xs, xs_b

            def stage2(r, st1):
                xs, xs_b = st1
                j = 0 if r < 32 else 1
                tm, tm_b = tmr.next()
                hf, hf_b = hfr.next()
                for hb in range(2):
                    pt, pt_b = self.pring.next()
                    for kk in range(4):
                        k = hb * 4 + kk
                        B.op(B.pe, lambda e: e.transpose(out=pt[:, kk * 128:(kk + 1) * 128], in_=xs[:, k * 128:(k + 1) * 128],
                                                         identity=self.ident[:]),
                             reads=[xs_b, self.ident_b], writes=[pt_b])
                    B.op(B.dve, lambda e: e.tensor_tensor(
                        out=tm[:, hb * 4:hb * 4 + 4, :], in0=pt[:, :].rearrange("p (k t) -> p k t", t=128),
                        in1=self.G[:, w, j, hb * 4:hb * 4 + 4].unsqueeze(2).to_broadcast([128, 4, 128]), op=ALU.mult),
                        reads=[pt_b, self.G_b], writes=[tm_b])
                    B.op(B.dve, lambda e: e.tensor_tensor(
                        out=hf[:, hb * 4:hb * 4 + 4, :], in0=tm[:, hb * 4:hb * 4 + 4, :],
                        in1=self.modT[:, sh0 + hb * 4:sh0 + hb * 4 + 4, j].unsqueeze(2).to_broadcast([128, 4, 128]), op=ALU.add),
                        reads=[tm_b, self.modT_b], writes=[hf_b])
                return hf, hf_b

            def stage3(r, st2):
                hf, hf_b = st2
                if which == 1:
                    lo = loc(s, r * 128)
                    B.op(B.act, lambda e: e.activation(out=self.hT[:, :, lo:lo + 128], in_=hf[:], func=AF.Copy),
                         reads=[hf_b], writes=[self.hT_b])
                else:
                    hb_, hb_b = hbr.next()
                    B.op(B.act, lambda e: e.activation(out=hb_[:], in_=hf[:], func=AF.Copy), reads=[hf_b], writes=[hb_b])
                    B.dma(B.sp, self.hT_d.rearrange("(k p) t -> p k t", p=128)[:, :, r * 128:(r + 1) * 128], hb_[:], reads=[hb_b])
                    self.router_tile(r, hf, hf_b)

            nt_ = len(tiles)
            q1, q2 = {}, {}
            for i in range(nt_ + 2):
                if i < nt_:
                    q1[i] = stage1(tiles[i])
                if 1 <= i <= nt_:
                    q2[i - 1] = stage2(tiles[i - 1], q1.pop(i - 1))
                if i >= 2:
                    stage3(tiles[i - 2], q2.pop(i - 2))
            B.barrier()

    def router_setup(self, st):
        B, Lw = self.B, self.Lw
        self.rtw, self.rtw_b = B.sb(st, [128, 8, NE], F32, "rtw")
        self.rtb, self.rtb_b = B.sb(st, [128, NE], F32, "rtb")
        B.dma(B.sp, self.rtw[:], Lw["rtw"].rearrange("(k p) e -> p k e", p=128), writes=[self.rtw_b])
        B.dma(B.sp, self.rtb[:], Lw["rtb"][:, :], writes=[self.rtb_b])
        self.rt_lg = B.ring(st, 3, [128, NE], F32, "lg")
        self.rt_t8 = B.ring(st, 3, [128, 8], F32, "t8")
        self.rt_sm = B.ring(st, 3, [128, 4], F32, "rsm")
        self.rt_mk = B.ring(st, 3, [128, NE], F32, "mk")
        self.rt_ex = B.ring(st, 3, [128, NE], F32, "ex")

    def router_tile(self, r, hf, hf_b):
        B = self.B
        ps, ps_b = self.pring.next()
        for k in range(8):
            B.op(B.pe, lambda e: e.matmul(ps[:, 0:NE], lhsT=hf[:, k, :], rhs=self.rtw[:, k, :], start=(k == 0), stop=(k == 7)),
                 reads=[hf_b, self.rtw_b], writes=[ps_b])
        lg, lg_b = self.rt_lg.next()
        t8, t8_b = self.rt_t8.next()
        sm, sm_b = self.rt_sm.next()
        mk, mk_b = self.rt_mk.next()
        ex, ex_b = self.rt_ex.next()
        B.op(B.dve, lambda e: e.tensor_tensor(out=lg[:], in0=ps[:, 0:NE], in1=self.rtb[:], op=ALU.add), reads=[ps_b, self.rtb_b], writes=[lg_b])
        B.op(B.dve, lambda e: e.max(out=t8[:], in_=lg[:]), reads=[lg_b], writes=[t8_b])
        B.op(B.dve, lambda e: e.tensor_scalar(out=mk[:], in0=lg[:], scalar1=t8[:, 3:4], scalar2=None, op0=ALU.is_ge),
             reads=[lg_b, t8_b], writes=[mk_b])
        B.op(B.dve, lambda e: e.tensor_scalar(out=sm[:, 0:1], in0=t8[:, 0:1], scalar1=-1.0, scalar2=None, op0=ALU.mult),
             reads=[t8_b], writes=[sm_b])
        B.op(B.act, lambda e: e.activation(out=ex[:], in_=lg[:], func=AF.Exp, bias=sm[:, 0:1], scale=1.0),
             reads=[lg_b, sm_b], writes=[ex_b])
        B.op(B.dve, lambda e: e.tensor_tensor(out=ex[:], in0=ex[:], in1=mk[:], op=ALU.mult), reads=[ex_b, mk_b], writes=[ex_b])
        B.op(B.dve, lambda e: e.reduce_sum(out=sm[:, 1:2], in_=ex[:], axis=AX.X), reads=[ex_b], writes=[sm_b])
        B.op(B.dve, lambda e: e.reciprocal(out=sm[:, 2:3], in_=sm[:, 1:2]), reads=[sm_b], writes=[sm_b])
        B.op(B.dve, lambda e: e.tensor_scalar(out=self.rw[:, r, :], in0=ex[:], scalar1=sm[:, 2:3], scalar2=None, op0=ALU.mult),
             reads=[ex_b, sm_b], writes=[self.rw_b])

    def phase_proj(self, s):
        B, nc, Lw = self.B, self.nc, self.Lw
        wsrc = Lw["win"].rearrange("(k p) n -> p k n", p=128)
        with contextlib.ExitStack() as st:
            wr = B.ring(st, 2, [128, 8, 512], BF16, "wp")
            bg, bg_b = B.sb(st, [128, 32], F32, "bg")
            qkg, qkg_b = B.sb(st, [128, 14], F32, "qkg")
            B.dma(B.sp, bg[:], Lw["bgate"][:, :], writes=[bg_b])
            B.dma(B.sp, qkg[:], Lw["qkg"][:, :], writes=[qkg_b])
            cs = {}
            tabsrc = (("cos64", self.c_cos64), ("sin64", self.c_sin64), ("cos32", self.c_cos32), ("sin32", self.c_sin32))
            for nm, srcap in tabsrc:
                cs[nm] = B.ring(st, 3, [128, 512], F32, nm)
            sqr = B.ring(st, 3, [128, 512], BF16, "sqq")
            rsr = B.ring(st, 3, [128, 512], F32, "rs")
            qnr = B.ring(st, 3, [128, 512], BF16, "qn")
            t1r = B.ring(st, 2, [128, 512], F32, "t1")
            t2r = B.ring(st, 2, [128, 512], F32, "t2")
            outr = B.ring(st, 4, [128, 512], BF16, "po")
            vr = B.ring(st, 2, [128, 768], BF16, "vsb")
            pieces = [(0, 512, "q", 0), (512, 512, "q", 4), (1024, 512, "k", 8), (1536, 256, "k", 12)]
            pieces += [(2560 + 512 * i, 512, "g", 4 * i) for i in range(8)]
            gsz = [64, 64, 32, 32, 64, 64, 64, 64, 64, 32, 32, 64, 64, 64]
            rope = [1, 1, 1, 1, 1, 1, 0, 0, 1, 1, 1, 1, 0, 0]
            for (c0, ncol, kind, ch0) in pieces:
                wp, wp_b = wr.next()
                B.dma(B.pool, wp[:, :, 0:ncol], wsrc[:, :, c0:c0 + ncol], writes=[wp_b])
                items = []
                for (t0, n) in half_blocks(s):
                    latent = t0 < HALF
                    lo = loc(s, t0)
                    if kind in "gq" and (not latent) and not self.need_ctx:
                        continue
                    ctab = {}
                    for i in range(ncol // 128):
                        items.append((t0, n, latent, lo, ctab, i))

                def stA(item):
                    t0, n, latent, lo, ctab, i = item
                    if i == 0 and kind != "g" and latent and c0 != 1536:
                        for nm, srcap in tabsrc:
                            t, b = cs[nm].next()
                            B.dma(B.sp, t[:, 0:n], srcap[:, t0:t0 + n], writes=[b])
                            ctab[nm] = (t, b)
                    ps, ps_b = self.pring.next()
                    for k in range(8):
                        B.op(B.pe, lambda e: e.matmul(ps[:, 0:n], lhsT=wp[:, k, i * 128:(i + 1) * 128], rhs=self.hT[:, k, lo:lo + n],
                                                      start=(k == 0), stop=(k == 7)),
                             reads=[wp_b, self.hT_b], writes=[ps_b])
                    if kind == "g":
                        po, po_b = outr.next()
                        gc = ch0 + i
                        B.op(B.act, lambda e: e.activation(out=po[:, 0:n], in_=ps[:, 0:n], func=AF.Sigmoid, bias=bg[:, gc:gc + 1]),
                             reads=[ps_b, bg_b], writes=[po_b])
                        B.dma(B.sp, self.gT_d[gc * 128:(gc + 1) * 128, t0:t0 + n], po[:, 0:n], reads=[po_b])
                        return None
                    sq, sq_b = sqr.next()
                    B.op(B.act, lambda e: e.activation(out=sq[:, 0:n], in_=ps[:, 0:n], func=AF.Square), reads=[ps_b], writes=[sq_b])
                    return (ps, ps_b, sq, sq_b)

                def stB1(item, sa):
                    t0, n, latent, lo, ctab, i = item
                    ps, ps_b, sq, sq_b = sa
                    ci = ch0 + i
                    gs = gsz[ci]
                    rs, rs_b = rsr.next()
                    blk, blk_b = self.cb["blk%d" % gs]
                    p2, p2_b = self.pring.next()
                    B.op(B.pe, lambda e: e.matmul(p2[:, 0:n], lhsT=blk[:], rhs=sq[:, 0:n], start=True, stop=True),
                         reads=[blk_b, sq_b], writes=[p2_b])
                    B.op(B.act, lambda e: e.activation(out=rs[:, 0:n], in_=p2[:, 0:n], func=AF.Sqrt, bias=EPS, scale=1.0),
                         reads=[p2_b], writes=[rs_b])
                    B.op(B.dve, lambda e: e.reciprocal(out=rs[:, 0:n], in_=rs[:, 0:n]), reads=[rs_b], writes=[rs_b])
                    dorope = rope[ci] and latent
                    if dorope:
                        dst, dst_b = qnr.next()
                    else:
                        dst, dst_b = outr.next()
                    B.op(B.dve, lambda e: e.scalar_tensor_tensor(out=dst[:, 0:n], in0=ps[:, 0:n], scalar=qkg[:, ci:ci + 1], in1=rs[:, 0:n],
                                                                 op0=ALU.mult, op1=ALU.mult),
                         reads=[ps_b, qkg_b, rs_b], writes=[dst_b])
                    return (dst, dst_b, dorope)

                def stB2(item, sb1):
                    t0, n, latent, lo, ctab, i = item
                    dst, dst_b, dorope = sb1
                    ci = ch0 + i
                    gs = gsz[ci]
                    if dorope:
                        qn, qn_b = dst, dst_b
                        po, po_b = outr.next()
                        rot, rot_b = self.cb["rot%d" % gs]
                        p3, p3_b = self.pring.next()
                        B.op(B.pe, lambda e: e.matmul(p3[:, 0:n], lhsT=rot[:], rhs=qn[:, 0:n], start=True, stop=True),
                             reads=[rot_b, qn_b], writes=[p3_b])
                        ct, ct_b = ctab["cos%d" % gs]
                        sn, sn_b = ctab["sin%d" % gs]
                        t1, t1_b = t1r.next()
                        t2, t2_b = t2r.next()
                        B.op(B.pool, lambda e: e.tensor_tensor(out=t1[:, 0:n], in0=qn[:, 0:n], in1=ct[:, 0:n], op=ALU.mult),
                             reads=[qn_b, ct_b], writes=[t1_b])
                        B.op(B.dve, lambda e: e.tensor_tensor(out=t2[:, 0:n], in0=p3[:, 0:n], in1=sn[:, 0:n], op=ALU.mult),
                             reads=[p3_b, sn_b], writes=[t2_b])
                        B.op(B.dve, lambda e: e.tensor_tensor(out=po[:, 0:n], in0=t1[:, 0:n], in1=t2[:, 0:n], op=ALU.add),
                             reads=[t1_b, t2_b], writes=[po_b])
                    else:
                        po, po_b = dst, dst_b
                    if kind == "q":
                        B.dma(B.sp, self.qT_d[ci * 128:(ci + 1) * 128, t0:t0 + n], po[:, 0:n], reads=[po_b])
                    else:
                        kc = ci - 8
                        if latent:
                            B.dma(B.sp, self.k_own(kc)[:, t0:t0 + n], po[:, 0:n], reads=[po_b])
                        else:
                            B.dma(B.sp, self.kctx[kc * 128:(kc + 1) * 128, 0:n], po[:, 0:n], reads=[po_b])

                ni = len(items)
                qa, qb = {}, {}
                for ii in range(ni + 2):
                    if ii < ni:
                        qa[ii] = stA(items[ii])
                    if kind == "g":
                        continue
                    if 1 <= ii <= ni:
                        qb[ii - 1] = stB1(items[ii - 1], qa.pop(ii - 1))
                    if ii >= 2:
                        stB2(items[ii - 2], qb.pop(ii - 2))
            wv, wv_b = B.sb(st, [128, 8, 768], BF16, "wv")
            B.dma(B.pool, wv[:], wsrc[:, :, 1792:2560], writes=[wv_b])
            for r in half_tiles(s):
                lo = loc(s, r * 128)
                pa, pa_b = self.pring.next()
                pb, pb_b = self.pring.next()
                for k in range(8):
                    B.op(B.pe, lambda e: e.matmul(pa[:, :], lhsT=self.hT[:, k, lo:lo + 128], rhs=wv[:, k, 0:512],
                                                  start=(k == 0), stop=(k == 7)), reads=[self.hT_b, wv_b], writes=[pa_b])
                for k in range(8):
                    B.op(B.pe, lambda e: e.matmul(pb[:, 0:256], lhsT=self.hT[:, k, lo:lo + 128], rhs=wv[:, k, 512:768],
                                                  start=(k == 0), stop=(k == 7)), reads=[self.hT_b, wv_b], writes=[pb_b])
                vs, vs_b = vr.next()
                B.op(B.act, lambda e: e.activation(out=vs[:, 0:512], in_=pa[:, :], func=AF.Copy), reads=[pa_b], writes=[vs_b])
                B.op(B.dve, lambda e: e.tensor_copy(out=vs[:, 512:768], in_=pb[:, 0:256]), reads=[pb_b], writes=[vs_b])
                if r < 32:
                    B.dma(B.sp, self.vsend[self.l][r // 8][(r % 8) * 128:(r % 8 + 1) * 128, :], vs[:], reads=[vs_b])
                else:
                    B.dma(B.sp, self.vctx[(r - 32) * 128:(r - 31) * 128, :], vs[:], reads=[vs_b])
            B.barrier()

    def phase_attn_setup(self):
        B, Lw = self.B, self.Lw
        lst = self.ast
        lam_init = 0.8 - 0.6 * math.exp(-0.3 * self.l)
        self.lam_init = lam_init
        self.neglam, self.neglam_b = B.sb(lst, [128, 4], F32, "neglam")
        self.esink, self.esink_b = B.sb(lst, [128, 4], F32, "esink")
        self.subg, self.subg_b = B.sb(lst, [128, 64], F32, "subg")
        self.dE, self.dE_b = B.sb(lst, [128, NPD * 4, 128], BF16, "dE")
        self.cM, self.cM_b = B.sb(lst, [128, 5, 128], BF16, "cM")
        with contextlib.ExitStack() as st:
            lv, lv_b = B.sb(st, [128, 4, 32], F32, "lv")
            pr, pr_b = B.sb(st, [128, 2, 32], F32, "pr")
            sm, sm_b = B.sb(st, [128, 4], F32, "lsm")
            B.dma(B.sp, lv[:], Lw["lamv"].rearrange("p (a b) -> p a b", b=32), writes=[lv_b])
            B.dma(B.sp, self.subg[:], Lw["subg"][:, :], writes=[self.subg_b])
            B.dma(B.sp, self.esink[:], Lw["sinkb"][:, :], writes=[self.esink_b])
            B.op(B.act, lambda e: e.activation(out=self.esink[:], in_=self.esink[:], func=AF.Exp), reads=[self.esink_b], writes=[self.esink_b])
            B.op(B.dve, lambda e: e.tensor_scalar(out=self.subg[:], in0=self.subg[:], scalar1=float(1.0 - lam_init), scalar2=None, op0=ALU.mult),
                 reads=[self.subg_b], writes=[self.subg_b])
            B.op(B.dve, lambda e: e.tensor_tensor(out=pr[:, 0, :], in0=lv[:, 0, :], in1=lv[:, 1, :], op=ALU.mult), reads=[lv_b], writes=[pr_b])
            B.op(B.dve, lambda e: e.tensor_tensor(out=pr[:, 1, :], in0=lv[:, 2, :], in1=lv[:, 3, :], op=ALU.mult), reads=[lv_b], writes=[pr_b])
            B.op(B.dve, lambda e: e.reduce_sum(out=sm[:, 0:2], in_=pr[:], axis=AX.X), reads=[pr_b], writes=[sm_b])
            B.op(B.act, lambda e: e.activation(out=sm[:, 2:4], in_=sm[:, 0:2], func=AF.Exp), reads=[sm_b], writes=[sm_b])
            B.op(B.dve, lambda e: e.tensor_tensor(out=sm[:, 0:1], in0=sm[:, 3:4], in1=sm[:, 2:3], op=ALU.subtract), reads=[sm_b], writes=[sm_b])
            B.op(B.dve, lambda e: e.tensor_scalar(out=self.neglam[:, 0:1], in0=sm[:, 0:1], scalar1=float(-lam_init), scalar2=None, op0=ALU.add),
                 reads=[sm_b], writes=[self.neglam_b])
            B.dma(B.pool, self.cM[:], self.c_cmask.rearrange("p (a b) -> p a b", b=128), writes=[self.cM_b])
            dm, dm_b = B.sb(st, [128, NPD, 128], F32, "dm")
            B.dma(B.sp, dm[:], self.c_dmask.rearrange("p (a b) -> p a b", b=128), writes=[dm_b])
            dtr = B.ring(st, 2, [128, 4, 128], F32, "dt")
            dsrc = Lw["dtab"].rearrange("p (a h b) -> p a h b", h=4, b=128)
            for pid in range(NPD):
                dt_, dt_b = dtr.next()
                B.dma(B.sp, dt_[:], dsrc[:, pid, :, :], writes=[dt_b])
                B.op(B.act, lambda e: e.activation(out=dt_[:], in_=dt_[:], func=AF.Exp), reads=[dt_b], writes=[dt_b])
                B.op(B.dve, lambda e: e.tensor_tensor(out=self.dE[:, pid * 4:(pid + 1) * 4, :], in0=dt_[:],
                                                      in1=dm[:, pid:pid + 1, :].to_broadcast([128, 4, 128]), op=ALU.mult),
                     reads=[dt_b, dm_b], writes=[self.dE_b])
            B.barrier()

    def k_own(self, c):
        return self.ksend[self.l][c // 2][(c % 2) * 128:(c % 2 + 1) * 128, :]

    def k_all(self, r, c):
        return self.kall[self.l][c // 2][r * 256 + (c % 2) * 128:r * 256 + (c % 2 + 1) * 128, :]

    def phase_gather(self):
        B = self.B
        l = self.l
        groups = [[2 * i, 2 * i + 1] for i in range(self.n_cores // 2)]
        B.barrier()
        for j in range(3):
            B.coll("AllGather", [self.ksend[l][j].opt()], [self.kall[l][j].opt()], groups)
        for j in range(4):
            B.coll("AllGather", [self.vsend[l][j].opt()], [self.vall[l][j].opt()], groups)
        B.barrier()

    def load_kv(self, st, mixer):
        B = self.B
        l = self.l
        kc0 = {"A": 0, "B": 1}[mixer]
        vc0 = {"A": 0, "B": 128}[mixer]
        nvh = {"A": 2, "B": 4}[mixer]
        nk = 2 if mixer == "A" else 3
        kT, kT_b = B.sb(st, [128, nk, TOKB], BF16, "kT" + mixer)
        srcs = [((lambda c, r=r: self.k_all(r, c)), r * HALF, HALF) for r in range(2)]
        srcs += [((lambda c: self.kctx[c * 128:(c + 1) * 128, :]), SEQ, CTX)]
        for (srcf, c0, n) in srcs:
            if mixer == "A":
                src = srcf(kc0)
                for kv in range(2):
                    for hf in range(2):
                        B.dma(B.sp, kT[hf * 64:(hf + 1) * 64, kv, c0:c0 + n], src[kv * 64:(kv + 1) * 64, :], writes=[kT_b])
            else:
                for c in range(2):
                    src = srcf(kc0 + c)
                    B.dma(B.sp, kT[:, c, c0:c0 + n], src[:, :], writes=[kT_b])
                    B.dma(B.sp, kT[32 * c:32 * c + 32, 2, c0:c0 + n], src[96:128, :], writes=[kT_b])
        va, va_b = B.sb(st, [128, NTB, nvh, 65], BF16, "va" + mixer)
        with contextlib.ExitStack() as st2:
            vs, vs_b = B.sb(st2, [128, NTB, nvh * 64], BF16, "vstage")
            for r in range(2):
                for j in range(4):
                    vsrc = self.vall[l][j].rearrange("(t p) c -> p t c", p=128)
                    B.dma(B.sp, vs[:, r * 32 + j * 8:r * 32 + j * 8 + 8, :], vsrc[:, r * 8:(r + 1) * 8, vc0:vc0 + nvh * 64], writes=[vs_b])
            B.dma(B.sp, vs[:, 64:66, :], self.vctx.rearrange("(t p) c -> p t c", p=128)[:, :, vc0:vc0 + nvh * 64], writes=[vs_b])
            B.op(B.pool, lambda e: e.memset(va[:, :, :, 64:65], 1.0), writes=[va_b])
            B.op(B.dve, lambda e: e.tensor_copy(out=va[:, :, :, 0:64], in_=vs[:].rearrange("p t (h d) -> p t h d", d=64)),
                 reads=[vs_b], writes=[va_b])
            B.barrier()
        return kT, kT_b, va, va_b

    def load_kv_local(self, st, mixer):
        B = self.B
        l = self.l
        kc0 = {"C": 3, "D": 4}[mixer]
        vc0 = {"C": 384, "D": 512}[mixer]
        nvh = {"C": 2, "D": 4}[mixer]
        NS = 38
        kT, kT_b = B.sb(st, [128, 2, NS * 128], BF16, "kT" + mixer)
        va, va_b = B.sb(st, [128, NS, nvh, 65], BF16, "va" + mixer)
        with contextlib.ExitStack() as st2:
            kO, kO_b = B.sb(st2, [128, 2, 2, 512], BF16, "kO")
            tk, tk_b = B.sb(st2, [128, 2, 512], F32, "tk")
            vs, vs_b = B.sb(st2, [128, NS, nvh * 64], BF16, "vstage")
            vO, vO_b = B.sb(st2, [128, 2, 4, nvh * 64], BF16, "vO")
            tv, tv_b = B.sb(st2, [128, 4, nvh * 64], F32, "tv")
            sel, sel_b = B.sb(st2, [128, 2], F32, "sel")
            B.dma(B.sp, sel[:], self.c_sel[:, :], writes=[sel_b])

            def kload(dst, dst_b, cols, srcf, scols):
                if mixer == "C":
                    src = srcf(kc0)
                    for kv in range(2):
                        for hf in range(2):
                            B.dma(B.sp, dst(hf * 64, (hf + 1) * 64, kv, cols), src[kv * 64:(kv + 1) * 64, scols[0]:scols[1]], writes=[dst_b])
                else:
                    for c in range(2):
                        src = srcf(kc0 + c)
                        B.dma(B.sp, dst(0, 128, c, cols), src[:, scols[0]:scols[1]], writes=[dst_b])

            kload(lambda p0, p1, c, cols: kT[p0:p1, c, cols[0]:cols[1]], kT_b, (0, HALF), self.k_own, (0, HALF))
            kload(lambda p0, p1, c, cols: kT[p0:p1, c, cols[0]:cols[1]], kT_b, (36 * 128, 38 * 128), (lambda c: self.kctx[c * 128:(c + 1) * 128, :]), (0, CTX))
            for r in range(2):
                srcf = (lambda c, r=r: self.k_all(r, c))
                kload(lambda p0, p1, c, cols: kO[p0:p1, c, r, cols[0]:cols[1]], kO_b, (0, 256), srcf, (0, 256))
                kload(lambda p0, p1, c, cols: kO[p0:p1, c, r, cols[0]:cols[1]], kO_b, (256, 512), srcf, (HALF - 256, HALF))
            B.op(B.dve, lambda e: e.tensor_scalar(out=tk[:], in0=kO[:, :, 0, :], scalar1=sel[:, 0:1], scalar2=None, op0=ALU.mult),
                 reads=[kO_b, sel_b], writes=[tk_b])
            B.op(B.dve, lambda e: e.scalar_tensor_tensor(out=kT[:, :, 32 * 128:36 * 128], in0=kO[:, :, 1, :], scalar=sel[:, 1:2], in1=tk[:],
                                                         op0=ALU.mult, op1=ALU.add),
                 reads=[kO_b, sel_b, tk_b], writes=[kT_b])
            for j in range(4):
                B.dma(B.sp, vs[:, j * 8:(j + 1) * 8, :], self.vsend[l][j].rearrange("(t p) c -> p t c", p=128)[:, :, vc0:vc0 + nvh * 64], writes=[vs_b])
            B.dma(B.sp, vs[:, 36:38, :], self.vctx.rearrange("(t p) c -> p t c", p=128)[:, :, vc0:vc0 + nvh * 64], writes=[vs_b])
            for r in range(2):
                v0 = self.vall[l][0].rearrange("(t p) c -> p t c", p=128)
                v3 = self.vall[l][3].rearrange("(t p) c -> p t c", p=128)
                B.dma(B.sp, vO[:, r, 0:2, :], v0[:, r * 8:r * 8 + 2, vc0:vc0 + nvh * 64], writes=[vO_b])
                B.dma(B.sp, vO[:, r, 2:4, :], v3[:, r * 8 + 6:r * 8 + 8, vc0:vc0 + nvh * 64], writes=[vO_b])
            B.op(B.dve, lambda e: e.tensor_scalar(out=tv[:], in0=vO[:, 0, :, :], scalar1=sel[:, 0:1], scalar2=None, op0=ALU.mult),
                 reads=[vO_b, sel_b], writes=[tv_b])
            B.op(B.dve, lambda e: e.scalar_tensor_tensor(out=vs[:, 32:36, :], in0=vO[:, 1, :, :], scalar=sel[:, 1:2], in1=tv[:],
                                                         op0=ALU.mult, op1=ALU.add),
                 reads=[vO_b, sel_b, tv_b], writes=[vs_b])
            B.op(B.pool, lambda e: e.memset(va[:, :, :, 64:65], 1.0), writes=[va_b])
            B.op(B.dve, lambda e: e.tensor_copy(out=va[:, :, :, 0:64], in_=vs[:].rearrange("p t (h d) -> p t h d", d=64)),
                 reads=[vs_b], writes=[va_b])
            B.barrier()
        return kT, kT_b, va, va_b

    def load_q(self, st, s, mixer):
        B = self.B
        qc0 = {"A": 0, "B": 2, "C": 4, "D": 6}[mixer]
        n = TOKL if self.need_ctx else HALF
        qT, qT_b = B.sb(st, [128, 3 if mixer == "B" else 2, TOKL], BF16, "qT" + mixer)
        for c in range(2):
            B.dma(B.sp, qT[:, c, 0:n], self.qT_d[(qc0 + c) * 128:(qc0 + c + 1) * 128, 0:n], writes=[qT_b])
            if mixer == "B":
                B.dma(B.sp, qT[32 * c:32 * c + 32, 2, 0:n], self.qT_d[(qc0 + c) * 128 + 96:(qc0 + c + 1) * 128, 0:n], writes=[qT_b])
        return qT, qT_b

    def attn_global(self, s, mixer):
        B = self.B
        br = {"A": 0, "B": 1}[mixer]
        scale = 0.125 if mixer == "A" else 32 ** -0.5
        with contextlib.ExitStack() as st:
            kT, kT_b, va, va_b = self.load_kv(st, mixer)
            qT, qT_b = self.load_q(st, s, mixer)
            pr = B.ring(st, 5, [128, 512], BF16, "P")
            osb = B.ring(st, 2, [128, 2, 4, 65], F32, "osb")
            obr = B.ring(st, 2, [128, 4, 256], BF16, "ob")
            rcr = B.ring(st, 2, [128, 2, 4, 1], F32, "rc")
            o0r = B.ring(st, 2, [128, 4, 64], F32, "o0")
            o1r = B.ring(st, 2, [128, 4, 64], F32, "o1")
            sqr = B.ring(st, 2, [128, 4, 64], F32, "osq")
            ssr = B.ring(st, 2, [128, 4, 2], F32, "oss")
            blocks = half_blocks(s, with_ctx=self.need_ctx)
            if "qblocks" in self.limit:
                blocks = [blocks[i] for i in self.limit["qblocks"] if i < len(blocks)]
            for (t0, n) in blocks:
                lo = loc(s, t0)
                nsb = n // 128
                keytiles = list(range(NTB)) if t0 < HALF else [64, 65]
                ob, ob_b = obr.next()
                for h in range(4):
                    nc_ = 1 if mixer == "A" else 2
                    obanks = [self.pring.next() for _ in range(nc_)]
                    items = [(ki, kt, c) for ki, kt in enumerate(keytiles) for c in range(nc_)]
                    DEPTH = 3
                    pend = []

                    def stage1(item):
                        ki, kt, c = item
                        if mixer == "A":
                            pb = 64 * (h % 2)
                            kap = kT[pb:pb + 64, h // 2, kt * 128:(kt + 1) * 128]
                            qap = qT[pb:pb + 64, h // 2, lo:lo + n]
                            vap = va[:, kt, h // 2, :]
                        else:
                            g = (h % 2) * 2 + c
                            slot, pb = (h // 2, 32 * g) if g < 3 else (2, 32 * (h // 2))
                            kap = kT[pb:pb + 32, slot, kt * 128:(kt + 1) * 128]
                            qap = qT[pb:pb + 32, slot, lo:lo + n]
                            vap = va[:, kt, h, :]
                        sp_, sp_b = self.pring.next()
                        while any(sp_b is ob_[1] for ob_ in obanks):
                            sp_, sp_b = self.pring.next()
                        B.op(B.pe, lambda e: e.matmul(sp_[:, 0:n], lhsT=kap, rhs=qap, start=True, stop=True),
                             reads=[kT_b, qT_b], writes=[sp_b])
                        P, P_b = pr.next()
                        B.op(B.act, lambda e: e.activation(out=P[:, 0:n], in_=sp_[:, 0:n], func=AF.Exp, scale=float(scale)),
                             reads=[sp_b], writes=[P_b])
                        return (P, P_b, vap)

                    def stage2(item, st1):
                        ki, kt, c = item
                        P, P_b, vap = st1
                        ot, ot_b = obanks[c]
                        for sb in range(nsb):
                            B.op(B.pe, lambda e: e.matmul(ot[:, sb * 65:(sb + 1) * 65], lhsT=P[:, sb * 128:(sb + 1) * 128], rhs=vap,
                                                          start=(ki == 0 and sb == 0), stop=(ki == len(keytiles) - 1),
                                                          skip_group_check=True),
                                 reads=[P_b, va_b], writes=[ot_b])

                    for i in range(len(items) + DEPTH):
                        if i < len(items):
                            pend.append(stage1(items[i]))
                        if i >= DEPTH:
                            stage2(items[i - DEPTH], pend.pop(0))
                    os_, os_b = osb.next()
                    rc, rc_b = rcr.next()
                    for c in range(nc_):
                        ot, ot_b = obanks[c]
                        B.op(B.act, lambda e: e.activation(out=os_[:, c, 0:nsb, :], in_=ot[:, 0:nsb * 65].rearrange("p (a b) -> p a b", b=65), func=AF.Copy),
                             reads=[ot_b], writes=[os_b])
                    B.op(B.dve, lambda e: e.reciprocal(out=rc[:, 0:nc_, 0:nsb, :], in_=os_[:, 0:nc_, 0:nsb, 64:65]), reads=[os_b], writes=[rc_b])
                    if mixer == "A":
                        B.op(B.dve, lambda e: e.tensor_tensor(out=ob[:, 0:nsb, h * 64:(h + 1) * 64], in0=os_[:, 0, 0:nsb, 0:64],
                                                              in1=rc[:, 0, 0:nsb, :].to_broadcast([128, nsb, 64]), op=ALU.mult),
                             reads=[os_b, rc_b], writes=[ob_b])
                    else:
                        o0, o0_b = o0r.next()
                        o1, o1_b = o1r.next()
                        sq, sq_b = sqr.next()
                        ss, ss_b = ssr.next()
                        B.op(B.dve, lambda e: e.tensor_tensor(out=o0[:, 0:nsb, :], in0=os_[:, 0, 0:nsb, 0:64],
                                                              in1=rc[:, 0, 0:nsb, :].to_broadcast([128, nsb, 64]), op=ALU.mult),
                             reads=[os_b, rc_b], writes=[o0_b])
                        B.op(B.dve, lambda e: e.tensor_tensor(out=o1[:, 0:nsb, :], in0=os_[:, 1, 0:nsb, 0:64],
                                                              in1=rc[:, 1, 0:nsb, :].to_broadcast([128, nsb, 64]), op=ALU.mult),
                             reads=[os_b, rc_b], writes=[o1_b])
                        B.op(B.dve, lambda e: e.scalar_tensor_tensor(out=o0[:, 0:nsb, :], in0=o1[:, 0:nsb, :], scalar=self.neglam[:, 0:1],
                                                                     in1=o0[:, 0:nsb, :], op0=ALU.mult, op1=ALU.add),
                             reads=[o0_b, o1_b, self.neglam_b], writes=[o0_b])
                        B.op(B.dve, lambda e: e.tensor_tensor(out=sq[:, 0:nsb, :], in0=o0[:, 0:nsb, :], in1=o0[:, 0:nsb, :], op=ALU.mult),
                             reads=[o0_b], writes=[sq_b])
                        B.op(B.dve, lambda e: e.reduce_sum(out=ss[:, 0:nsb, 0:1], in_=sq[:, 0:nsb, :], axis=AX.X), reads=[sq_b], writes=[ss_b])
                        B.op(B.act, lambda e: e.activation(out=ss[:, 0:nsb, 1:2], in_=ss[:, 0:nsb, 0:1], func=AF.Sqrt, bias=EPS, scale=1.0 / 64),
                             reads=[ss_b], writes=[ss_b])
                        B.op(B.dve, lambda e: e.reciprocal(out=ss[:, 0:nsb, 0:1], in_=ss[:, 0:nsb, 1:2]), reads=[ss_b], writes=[ss_b])
                        B.op(B.dve, lambda e: e.tensor_tensor(out=o0[:, 0:nsb, :], in0=o0[:, 0:nsb, :],
                                                              in1=ss[:, 0:nsb, 0:1].to_broadcast([128, nsb, 64]), op=ALU.mult),
                             reads=[o0_b, ss_b], writes=[o0_b])
                        B.op(B.dve, lambda e: e.tensor_tensor(out=ob[:, 0:nsb, h * 64:(h + 1) * 64], in0=o0[:, 0:nsb, :],
                                                              in1=self.subg[:].unsqueeze(1).to_broadcast([128, nsb, 64]), op=ALU.mult),
                             reads=[o0_b, self.subg_b], writes=[ob_b])
                B.dma(B.sp, self.br_d.rearrange("(t p) c -> p t c", p=128)[:, t0 // 128:t0 // 128 + nsb, br * 256:(br + 1) * 256],
                      ob[:, 0:nsb, :], reads=[ob_b])
            B.barrier()

    def attn_local(self, s, mixer):
        B = self.B
        br = {"C": 2, "D": 3}[mixer]
        scale = 0.125
        with contextlib.ExitStack() as st:
            kT, kT_b, va, va_b = self.load_kv_local(st, mixer)
            qT, qT_b = self.load_q(st, s, mixer)
            pr = B.ring(st, 3, [128, 8, 128], BF16, "PL")
            osb = B.ring(st, 2, [128, 4, 65], F32, "osbl")
            obr = B.ring(st, 2, [128, 256], BF16, "obl")
            rcr = B.ring(st, 2, [128, 4, 1], F32, "rcl")
            tiles = half_tiles(s, with_ctx=self.need_ctx)
            if "ltiles" in self.limit:
                tiles = [t for t in tiles if t in self.limit["ltiles"]]
            for g in tiles:
                lo = loc(s, g * 128)
                if g >= 32:
                    offs = []
                else:
                    offs = l_offsets(g, mixer)
                local = [lslot(g + o) for o in offs]
                kts = local + [36, 37]
                nl = len(local)
                ot, ot_b = self.pring.next()
                def stage1(h):
                    pb = 64 * (h % 2)
                    kcol = h // 2
                    qap = qT[pb:pb + 64, h // 2, lo:lo + 128]
                    sa, sa_b = self.pring.next()
                    while sa_b is ot_b:
                        sa, sa_b = self.pring.next()
                    sb_, sb_b = self.pring.next()
                    while sb_b is ot_b or sb_b is sa_b:
                        sb_, sb_b = self.pring.next()
                    P, P_b = pr.next()
                    for j, kt in enumerate(kts):
                        bank, bank_b = (sa, sa_b) if j < 4 else (sb_, sb_b)
                        jj = j % 4
                        kap = kT[pb:pb + 64, kcol, kt * 128:(kt + 1) * 128]
                        B.op(B.pe, lambda e: e.matmul(bank[:, jj * 128:(jj + 1) * 128], lhsT=kap, rhs=qap, start=True, stop=True,
                                                      skip_group_check=True),
                             reads=[kT_b, qT_b], writes=[bank_b])
                    n1 = min(4, len(kts))
                    B.op(B.act, lambda e: e.activation(out=P[:, 0:n1, :], in_=sa[:, 0:n1 * 128].rearrange("p (a b) -> p a b", b=128),
                                                       func=AF.Exp, scale=float(scale)), reads=[sa_b], writes=[P_b])
                    if len(kts) > 4:
                        n2 = len(kts) - 4
                        B.op(B.act, lambda e: e.activation(out=P[:, 4:4 + n2, :], in_=sb_[:, 0:n2 * 128].rearrange("p (a b) -> p a b", b=128),
                                                           func=AF.Exp, scale=float(scale)), reads=[sb_b], writes=[P_b])
                    for j, o in enumerate(offs):
                        if mixer == "C":
                            tab = self.cM[:, l_pid(g, o, "C"), :]
                            tab_b = self.cM_b
                        else:
                            tab = self.dE[:, l_pid(g, o, "D") * 4 + h, :]
                            tab_b = self.dE_b
                        B.op(B.dve, lambda e: e.tensor_tensor(out=P[:, j, :], in0=P[:, j, :], in1=tab, op=ALU.mult),
                             reads=[P_b, tab_b], writes=[P_b])
                    return P, P_b

                def stage2(h, st1):
                    P, P_b = st1
                    kv = h // 2 if mixer == "C" else h
                    for j, kt in enumerate(kts):
                        B.op(B.pe, lambda e: e.matmul(ot[:, h * 65:(h + 1) * 65], lhsT=P[:, j, :], rhs=va[:, kt, kv, :],
                                                      start=(j == 0 and h == 0), stop=(j == len(kts) - 1), skip_group_check=True),
                             reads=[P_b, va_b], writes=[ot_b])

                DEPTH = 2
                pend = []
                for i in range(4 + DEPTH):
                    if i < 4:
                        pend.append(stage1(i))
                    if i >= DEPTH:
                        stage2(i - DEPTH, pend.pop(0))
                os_, os_b = osb.next()
                rc, rc_b = rcr.next()
                ob, ob_b = obr.next()
                B.op(B.act, lambda e: e.activation(out=os_[:], in_=ot[:, 0:260].rearrange("p (a b) -> p a b", b=65), func=AF.Copy),
                     reads=[ot_b], writes=[os_b])
                if mixer == "C":
                    B.op(B.dve, lambda e: e.tensor_tensor(out=rc[:], in0=os_[:, :, 64:65], in1=self.esink[:].unsqueeze(2), op=ALU.add),
                         reads=[os_b, self.esink_b], writes=[rc_b])
                    B.op(B.dve, lambda e: e.reciprocal(out=rc[:], in_=rc[:]), reads=[rc_b], writes=[rc_b])
                else:
                    B.op(B.dve, lambda e: e.reciprocal(out=rc[:], in_=os_[:, :, 64:65]), reads=[os_b], writes=[rc_b])
                B.op(B.dve, lambda e: e.tensor_tensor(out=ob[:].rearrange("p (h d) -> p h d", d=64), in0=os_[:, :, 0:64],
                                                      in1=rc[:].to_broadcast([128, 4, 64]), op=ALU.mult),
                     reads=[os_b, rc_b], writes=[ob_b])
                B.dma(B.sp, self.br_d[g * 128:(g + 1) * 128, br * 256:(br + 1) * 256], ob[:], reads=[ob_b])
            B.barrier()
    def phase_merge(self, s):
        B, Lw = self.B, self.Lw
        xsrc = self.xin if self.l == 0 else self.xcur
        with contextlib.ExitStack() as st:
            wbr, wbr_b = B.sb(st, [128, 8, D], BF16, "wbr")
            wout, wout_b = B.sb(st, [128, 8, D], BF16, "wout")
            B.dma(B.pool, wbr[:], Lw["wbr"].rearrange("(k p) n -> p k n", p=128), writes=[wbr_b])
            B.dma(B.pool, wout[:], Lw["wout"].rearrange("(k p) n -> p k n", p=128), writes=[wout_b])
            brr = B.ring(st, 2, [128, D], BF16, "brt")
            brT, brT_b = B.sb(st, [128, 8, 512], BF16, "brT")
            gtr = B.ring(st, 2, [128, 8, 512], BF16, "gt")
            mg, mg_b = B.sb(st, [128, 8, 512], F32, "mg")
            mgb, mgb_b = B.sb(st, [128, 8, 512], BF16, "mgb")
            tpr = B.ring(st, 2, [128, 512], F32, "tp")
            xr = B.ring(st, 2, [128, D], F32, "xm")
            tr = B.ring(st, 2, [128, D], F32, "tmx")
            gsrc = self.gT_d.rearrange("(c p) t -> p c t", p=128)
            mblocks = half_blocks(s, with_ctx=self.need_ctx)
            if "mblocks" in self.limit:
                mblocks = [mblocks[i] for i in self.limit["mblocks"]]
            for (t0, n) in mblocks:
                nsb = n // 128
                j = 0 if t0 < HALF else 1
                for sb in range(nsb):
                    bt, bt_b = brr.next()
                    r = t0 // 128 + sb
                    B.dma(B.sp, bt[:], self.br_d[r * 128:(r + 1) * 128, :], writes=[bt_b])
                    pt, pt_b = self.pring.next()
                    ptb = pt[:, :].bitcast(BF16)
                    for k in range(8):
                        B.op(B.pe, lambda e: e.transpose(out=ptb[:, k * 128:(k + 1) * 128], in_=bt[:, k * 128:(k + 1) * 128], identity=self.identb[:]),
                             reads=[bt_b, self.identb_b], writes=[pt_b])
                    B.op(B.act, lambda e: e.activation(out=brT[:, :, sb * 128:(sb + 1) * 128], in_=ptb.rearrange("p (k t) -> p k t", t=128), func=AF.Copy),
                         reads=[pt_b], writes=[brT_b])
                for nb in range(4):
                    gt, gt_b = gtr.next()
                    B.dma(B.sp, gt[:, :, 0:n], gsrc[:, nb * 8:(nb + 1) * 8, t0:t0 + n], writes=[gt_b])
                    for dc in range(8):
                        ps, ps_b = self.pring.next()
                        for kk in range(2):
                            B.op(B.pe, lambda e: e.matmul(ps[:, 0:n], lhsT=wbr[:, nb * 2 + kk, dc * 128:(dc + 1) * 128], rhs=brT[:, nb * 2 + kk, 0:n],
                                                          start=(kk == 0), stop=(kk == 1)), reads=[wbr_b, brT_b], writes=[ps_b])
                        if nb == 0:
                            B.op(B.dve, lambda e: e.tensor_tensor(out=mg[:, dc, 0:n], in0=ps[:, 0:n], in1=gt[:, dc, 0:n], op=ALU.mult),
                                 reads=[ps_b, gt_b], writes=[mg_b])
                        else:
                            tp, tp_b = tpr.next()
                            B.op(B.dve, lambda e: e.tensor_tensor(out=tp[:, 0:n], in0=ps[:, 0:n], in1=gt[:, dc, 0:n], op=ALU.mult),
                                 reads=[ps_b, gt_b], writes=[tp_b])
                            B.op(B.pool, lambda e: e.tensor_tensor(out=mg[:, dc, 0:n], in0=mg[:, dc, 0:n], in1=tp[:, 0:n], op=ALU.add),
                                 reads=[mg_b, tp_b], writes=[mg_b])
                B.op(B.act, lambda e: e.activation(out=mgb[:, :, 0:n], in_=mg[:, :, 0:n], func=AF.Copy), reads=[mg_b], writes=[mgb_b])
                gt_, g_b = self.gbc[(1, j)]
                for sb in range(nsb):
                    r = t0 // 128 + sb
                    xt, xt_b = xr.next()
                    tm, tm_b = tr.next()
                    B.dma(B.sp, xt[:], xsrc[r * 128:(r + 1) * 128, :], writes=[xt_b])
                    for hh in range(2):
                        ps, ps_b = self.pring.next()
                        for k in range(8):
                            B.op(B.pe, lambda e: e.matmul(ps[:, :], lhsT=mgb[:, k, sb * 128:(sb + 1) * 128], rhs=wout[:, k, hh * 512:(hh + 1) * 512],
                                                          start=(k == 0), stop=(k == 7)), reads=[mgb_b, wout_b], writes=[ps_b])
                        B.op(B.dve, lambda e: e.tensor_tensor(out=tm[:, hh * 512:(hh + 1) * 512], in0=ps[:, :], in1=gt_[:, hh * 512:(hh + 1) * 512], op=ALU.mult),
                             reads=[ps_b, g_b], writes=[tm_b])
                    B.op(B.pool, lambda e: e.tensor_tensor(out=tm[:], in0=tm[:], in1=xt[:], op=ALU.add), reads=[tm_b, xt_b], writes=[tm_b])
                    B.dma(B.sp, self.xcur[r * 128:(r + 1) * 128, :], tm[:], reads=[tm_b])
            B.barrier()

    def phase_moe(self):
        B, Lw = self.B, self.Lw
        last = self.l == 1
        ntiles = NTL if self.need_ctx else 32
        passes = []
        t = 0
        sizes = [10, 8, 8, 8] if ntiles == NTL else [8] * 4
        for sz in sizes:
            passes.append(list(range(t, t + sz)))
            t += sz
        if "moe_tiles" in self.limit:
            passes = [list(self.limit["moe_tiles"])]
        experts = self.limit.get("experts", list(range(NE)))
        wgu = Lw["wgu"].rearrange("(e k p) n -> p e k n", p=128, k=8)
        wdn = Lw["wdn"].rearrange("(e k p) n -> p e k n", p=128, k=8)
        hsrc = self.hT_d.rearrange("(k p) t -> p k t", p=128)
        with contextlib.ExitStack() as st:
            bgu, bgu_b = B.sb(st, [128, NE * 16], F32, "bgu")
            B.dma(B.sp, bgu[:], Lw["bgu"][:, :], writes=[bgu_b])
            bdn, bdn_b = B.sb(st, [NE, D], F32, "bdn")
            B.dma(B.sp, bdn[:], Lw["bdn"][:, :], writes=[bdn_b])
            wring = B.ring(st, 8, [128, 8, 512], BF16, "wx")
            hT, hT_b = B.sb(st, [128, 8, 10 * 128], BF16, "hTm")
            acc, acc_b = B.sb(st, [128, 10, D], F32, "acc")
            actr = B.ring(st, 2, [128, 8, 512], BF16, "actT")
            gr = B.ring(st, 2, [128, 512], F32, "gq")
            sgr = B.ring(st, 2, [128, 512], F32, "sg")
            ur = B.ring(st, 2, [128, 512], F32, "uq")
            xr = B.ring(st, 2, [128, D], F32, "xo")
            rwT, rwT_b = B.sb(st, [NE, 128], F32, "rwT")
            for tiles in passes:
                nt = len(tiles)
                tok0 = tiles[0] * 128
                ntok = nt * 128
                B.dma(B.sp, hT[:, :, 0:ntok], hsrc[:, :, tok0:tok0 + ntok], writes=[hT_b])
                B.op(B.pool, lambda e: e.memset(acc[:, 0:nt, :], 0.0), writes=[acc_b])
                blocks = []
                o = 0
                while o < nt:
                    nb_ = min(4, nt - o)
                    blocks.append((o, nb_))
                    o += nb_
                for e_ in experts:
                    pcs = []
                    for pi in range(4):
                        wt, wt_b = wring.next()
                        B.dma(B.pool, wt[:], wgu[:, e_, :, pi * 512:(pi + 1) * 512], writes=[wt_b])
                        pcs.append((wt, wt_b))
                    dpc = []
                    for pi in range(2):
                        wt, wt_b = wring.next()
                        B.dma(B.pool, wt[:], wdn[:, e_, :, pi * 512:(pi + 1) * 512], writes=[wt_b])
                        dpc.append((wt, wt_b))
                    for (o, nb_) in blocks:
                        n = nb_ * 128
                        aT, aT_b = actr.next()
                        for fc in range(8):
                            gw, gw_b = pcs[fc // 4]
                            uw, uw_b = pcs[2 + fc // 4]
                            off = (fc % 4) * 128
                            pg, pg_b = self.pring.next()
                            pu, pu_b = self.pring.next()
                            for k in range(8):
                                B.op(B.pe, lambda e: e.matmul(pg[:, 0:n], lhsT=gw[:, k, off:off + 128], rhs=hT[:, k, o * 128:o * 128 + n],
                                                              start=(k == 0), stop=(k == 7)), reads=[gw_b, hT_b], writes=[pg_b])
                            for k in range(8):
                                B.op(B.pe, lambda e: e.matmul(pu[:, 0:n], lhsT=uw[:, k, off:off + 128], rhs=hT[:, k, o * 128:o * 128 + n],
                                                              start=(k == 0), stop=(k == 7)), reads=[uw_b, hT_b], writes=[pu_b])
                            g_, g_b = gr.next()
                            sg, sg_b = sgr.next()
                            u_, u_b = ur.next()
                            bgc = e_ * 16 + fc
                            buc = e_ * 16 + 8 + fc
                            B.op(B.dve, lambda e: e.tensor_scalar(out=g_[:, 0:n], in0=pg[:, 0:n], scalar1=bgu[:, bgc:bgc + 1], scalar2=7.0,
                                                                  op0=ALU.add, op1=ALU.min), reads=[pg_b, bgu_b], writes=[g_b])
                            B.op(B.act, lambda e: e.activation(out=sg[:, 0:n], in_=g_[:, 0:n], func=AF.Sigmoid, scale=1.702),
                                 reads=[g_b], writes=[sg_b])
                            B.op(B.dve, lambda e: e.tensor_scalar(out=u_[:, 0:n], in0=pu[:, 0:n], scalar1=bgu[:, buc:buc + 1], scalar2=7.0,
                                                                  op0=ALU.add, op1=ALU.min), reads=[pu_b, bgu_b], writes=[u_b])
                            B.op(B.dve, lambda e: e.tensor_scalar(out=u_[:, 0:n], in0=u_[:, 0:n], scalar1=-7.0, scalar2=1.0,
                                                                  op0=ALU.max, op1=ALU.add), reads=[u_b], writes=[u_b])
                            B.op(B.dve, lambda e: e.tensor_tensor(out=g_[:, 0:n], in0=g_[:, 0:n], in1=sg[:, 0:n], op=ALU.mult),
                                 reads=[g_b, sg_b], writes=[g_b])
                            B.op(B.dve, lambda e: e.tensor_tensor(out=aT[:, fc, 0:n], in0=g_[:, 0:n], in1=u_[:, 0:n], op=ALU.mult),
                                 reads=[g_b, u_b], writes=[aT_b])
                        for sb in range(nb_):
                            ti = o + sb
                            r = tiles[ti]
                            for hh in range(2):
                                dw, dw_b = dpc[hh]
                                py, py_b = self.pring.next()
                                for k in range(8):
                                    B.op(B.pe, lambda e: e.matmul(py[:, :], lhsT=aT[:, k, sb * 128:(sb + 1) * 128], rhs=dw[:, k, :],
                                                                  start=(k == 0), stop=(k == 7)), reads=[aT_b, dw_b], writes=[py_b])
                                B.op(B.dve, lambda e: e.scalar_tensor_tensor(out=acc[:, ti, hh * 512:(hh + 1) * 512], in0=py[:, :],
                                                                             scalar=self.rw[:, r, e_:e_ + 1], in1=acc[:, ti, hh * 512:(hh + 1) * 512],
                                                                             op0=ALU.mult, op1=ALU.add),
                                     reads=[py_b, self.rw_b, acc_b], writes=[acc_b])
                for ti, r in enumerate(tiles):
                    j = 0 if r < 32 else 1
                    pt, pt_b = self.pring.next()
                    B.op(B.pe, lambda e: e.transpose(out=pt[0:NE, 0:128], in_=self.rw[:, r, :], identity=self.ident[:]),
                         reads=[self.rw_b, self.ident_b], writes=[pt_b])
                    B.op(B.act, lambda e: e.activation(out=rwT[:], in_=pt[0:NE, 0:128], func=AF.Copy), reads=[pt_b], writes=[rwT_b])
                    xt, xt_b = xr.next()
                    B.dma(B.sp, xt[:], self.xcur[r * 128:(r + 1) * 128, :], writes=[xt_b])
                    gt_, g_b = self.gbc[(2, j)]
                    for hh in range(2):
                        py, py_b = self.pring.next()
                        B.op(B.pe, lambda e: e.matmul(py[:, :], lhsT=rwT[:], rhs=bdn[:, hh * 512:(hh + 1) * 512], start=True, stop=True),
                             reads=[rwT_b, bdn_b], writes=[py_b])
                        B.op(B.dve, lambda e: e.tensor_tensor(out=acc[:, ti, hh * 512:(hh + 1) * 512], in0=py[:, :],
                                                              in1=acc[:, ti, hh * 512:(hh + 1) * 512], op=ALU.add),
                             reads=[py_b, acc_b], writes=[acc_b])
                    B.op(B.dve, lambda e: e.tensor_tensor(out=acc[:, ti, :], in0=acc[:, ti, :], in1=gt_[:], op=ALU.mult),
                         reads=[acc_b, g_b], writes=[acc_b])
                    B.op(B.dve, lambda e: e.tensor_tensor(out=xt[:], in0=xt[:], in1=acc[:, ti, :], op=ALU.add),
                         reads=[xt_b, acc_b], writes=[xt_b])
                    if last:
                        B.dma(B.sp, self.y[r * 128:(r + 1) * 128, :], xt[:], reads=[xt_b])
                    else:
                        B.dma(B.sp, self.xcur[r * 128:(r + 1) * 128, :], xt[:], reads=[xt_b])
            B.barrier()
def rope_tabs(s, dim):
    t = np.arange(s * HALF, (s + 1) * HALF, dtype=np.int32)
    row = (t // GRID_W).astype(np.float32)
    col = (t % GRID_W).astype(np.float32)
    nf = dim // 4
    inv = (np.float32(10000.0) ** (-np.arange(nf, dtype=np.float32) / np.float32(nf))).astype(np.float32)
    ang = np.concatenate([row[:, None] * inv, col[:, None] * inv], axis=-1).astype(np.float32)
    cos = np.cos(ang).astype(np.float32)
    sin = np.sin(ang).astype(np.float32)
    f = np.arange(128) % (dim // 2)
    return np.ascontiguousarray(cos[:, f].T), np.ascontiguousarray(sin[:, f].T)


def d_patterns():
    pats = [(10, o) for o in range(-2, 3)]
    pats += [(0, o) for o in range(-2, 4)]
    pats += [(1, o) for o in range(-2, 3)]
    pats += [(30, o) for o in range(-2, 3)]
    pats += [(31, o) for o in range(-3, 3)]
    assert len(pats) == NPD
    return pats


def d_index(s):
    kp = np.arange(128)[:, None]
    qf = np.arange(128)[None, :]
    dr_all, dc_all, mk_all = [], [], []
    for (it, o) in d_patterns():
        g = 32 * s + it
        kt = g + o
        kr = 2 * kt + kp // 64
        kc = kp % 64
        qr = 2 * g + qf // 64
        qc = qf % 64
        r0 = np.clip(qr - 4, 0, 120)
        vrow = (kr >= r0) & (kr <= r0 + 7) & (kr >= 0) & (kr < 128)
        c0 = np.clip(qc - 8, 0, 48)
        vcol = (kc >= c0) & (kc < c0 + 16)
        dr = np.clip(kr - qr + 7, 0, 14)
        dc = np.clip(kc - qc, -15, 15) + 15
        dr_all.append(np.broadcast_to(dr, (128, 128)))
        dc_all.append(np.broadcast_to(dc, (128, 128)))
        mk_all.append(np.broadcast_to(vrow & vcol, (128, 128)))
    return np.stack(dr_all), np.stack(dc_all), np.stack(mk_all)


def consts_host(s):
    c = {}
    c["ident"] = np.eye(128, dtype=np.float32)
    for gs in (64, 32):
        g = np.arange(128) // gs
        c["blk%d" % gs] = (g[:, None] == g[None, :]).astype(np.float32) / np.float32(gs)
        rot = np.zeros((128, 128), np.float32)
        h = gs // 2
        for m in range(128):
            if m % gs < h:
                rot[m + h, m] = -1.0
            else:
                rot[m - h, m] = 1.0
        c["rot%d" % gs] = rot
        co, si = rope_tabs(s, gs)
        c["cos%d" % gs] = co
        c["sin%d" % gs] = si
    kp = np.arange(128)[:, None]
    qf = np.arange(128)[None, :]
    band = [(np.abs(qf - kp - 128 * o) <= 128).astype(np.float32) for o in (-1, 0, 1)]
    zero = np.zeros((128, 128), np.float32)
    cm = np.stack(band + [band[0] if s == 1 else zero, band[2] if s == 0 else zero], axis=1)
    c["cmask"] = cm.reshape(128, 5 * 128)
    _, _, mk = d_index(s)
    c["dmask"] = np.ascontiguousarray(mk.transpose(1, 0, 2).astype(np.float32)).reshape(128, NPD * 128)
    sel = np.zeros((128, 2), np.float32)
    sel[:, 1 - s] = 1.0
    c["sel"] = sel
    return c


def fm(v):
    v = np.asarray(v, np.float32)
    return np.ascontiguousarray(v.reshape(-1, 128).T)


def rep(v):
    v = np.asarray(v, np.float32).reshape(1, -1)
    return np.ascontiguousarray(np.broadcast_to(v, (128, v.shape[1])))


def prep_inputs(inp, prog, n_cores=8):
    shared = {}
    for l in range(2):
        sfx = "_%d" % l
        shared["wada" + sfx] = inp["w_ada"][l]
        shared["bada" + sfx] = fm(inp["b_ada"][l])
        shared["badarow" + sfx] = inp["b_ada"][l][None, :]
        shared["n1g" + sfx] = fm(inp["norm1_g"][l])
        shared["n2g" + sfx] = fm(inp["norm2_g"][l])
        shared["win" + sfx] = inp["w_in"][l]
        shared["bgate" + sfx] = fm(inp["b_gate"][l])
        t64 = lambda v: np.tile(v, 2)
        t32 = lambda v: np.tile(v, 4)
        cols = [t64(inp["a_qn"][l])] * 2 + [t32(inp["b_qn"][l])] * 2 + [t64(inp["c_qn"][l])] * 2 + [t64(inp["d_qn"][l])] * 2
        cols += [t64(inp["a_kn"][l])] + [t32(inp["b_kn"][l])] * 2 + [t64(inp["c_kn"][l])] + [t64(inp["d_kn"][l])] * 2
        shared["qkg" + sfx] = np.ascontiguousarray(np.stack(cols, axis=1).astype(np.float32))
        shared["lamv" + sfx] = rep(np.concatenate([inp["lam_q1"][l], inp["lam_k1"][l], inp["lam_q2"][l], inp["lam_k2"][l]]))
        shared["subg" + sfx] = rep(inp["subln_g"][l])
        shared["sinkb" + sfx] = rep(inp["sink"][l])
        shared["wbr" + sfx] = inp["w_branch"][l].reshape(D, D)
        shared["wout" + sfx] = inp["w_out"][l]
        shared["rtw" + sfx] = inp["router_w"][l]
        shared["rtb" + sfx] = rep(inp["router_b"][l])
        shared["wgu" + sfx] = inp["w_gate_up"][l].reshape(NE * D, 2 * D)
        shared["bgu" + sfx] = np.ascontiguousarray(inp["b_gate_up"][l].reshape(NE, 16, 128).transpose(2, 0, 1)).reshape(128, NE * 16)
        shared["wdn" + sfx] = inp["w_down"][l].reshape(NE * D, D)
        shared["bdn" + sfx] = inp["b_down"][l]
    shared = {k: np.ascontiguousarray(v, dtype=np.float32) for k, v in shared.items() if k in prog.ins}
    half = []
    for s in range(2):
        h = consts_host(s)
        dr, dc, _ = d_index(s)
        for l in range(2):
            tab = inp["rpb"][l][:, dr, dc]
            h["dtab_%d" % l] = np.ascontiguousarray(tab.transpose(2, 1, 0, 3)).reshape(128, NPD * 4 * 128)
        half.append({k: np.ascontiguousarray(v, dtype=np.float32) for k, v in h.items() if k in prog.ins})
    maps = []
    for c in range(n_cores):
        b, s = c // 2, c % 2
        m = dict(shared)
        m.update(half[s])
        m["xin"] = np.ascontiguousarray(np.concatenate([inp["x"][b, s * HALF:(s + 1) * HALF], inp["ctx"][b]], axis=0))
        m["cvec"] = np.ascontiguousarray(np.stack([fm(inp["c"][b]), fm(inp["c_ctx"])], axis=-1).reshape(128, 16))
        maps.append(m)
    return maps


def kernel(**inputs):
    inp = {k: np.asarray(v) for k, v in inputs.items()}
    prog = Prog()
    nc = prog.build()
    maps = prep_inputs(inp, prog)
    res = run_bass_kernel_spmd(nc, maps, core_ids=list(range(8)))
    out = np.zeros((4, SEQ, D), np.float32)
    for c in range(8):
        b, s = c // 2, c % 2
        out[b, s * HALF:(s + 1) * HALF] = res.results[c]["y"]
    return out
```

```python
import contextlib
import math
import numpy as np
import ml_dtypes
import concourse.bass as bass
import concourse.mybir as mybir
from concourse.bass_utils import run_bass_kernel_spmd

F32 = mybir.dt.float32
BF16 = mybir.dt.bfloat16
AF = mybir.ActivationFunctionType
ALU = mybir.AluOpType
AX = mybir.AxisListType

D = 1024
SEQ = 8192
HALF = 4096
CTX = 256
NT = 34
TOK = NT * 128
NE = 32
EPS = 1e-6
GRID_W = 64
TBS = [(i * 512, 512) for i in range(8)] + [(4096, 256)]
NPAT = 27


class Sem:
    def __init__(self, sem, name):
        self.sem = sem
        self.cnt = 0
        self.name = name


class Eng:
    def __init__(self, name, eng, sem):
        self.name = name
        self.eng = eng
        self.s = sem
        self.waited = {}


class Buf:
    __slots__ = ("w", "r", "name")

    def __init__(self, name=""):
        self.w = None
        self.r = {}
        self.name = name


class Builder:
    def __init__(self, nc, st):
        self.nc = nc
        self.st = st
        mk = lambda n: Sem(st.enter_context(nc.semaphore(n)), n)
        self.pe = Eng("pe", nc.tensor, mk("s_pe"))
        self.act = Eng("act", nc.scalar, mk("s_act"))
        self.dve = Eng("dve", nc.vector, mk("s_dve"))
        self.pool = Eng("pool", nc.gpsimd, mk("s_pool"))
        self.sp = Eng("sp", nc.sync, mk("s_sp"))
        self.engs = [self.pe, self.act, self.dve, self.pool, self.sp]
        self.dsems_hw = [mk("s_d%d" % i) for i in range(16)]
        self.dsems_sw = [mk("s_w%d" % i) for i in range(8)]
        self.dsems = self.dsems_hw + self.dsems_sw
        self.dnext = {"hw": 0, "sw": 0}
        self.csem = mk("s_cc")
        self.uid = 0

    def _wait(self, E, S, v):
        if v <= 0:
            return
        if E.waited.get(id(S), 0) >= v:
            return
        E.eng.wait_ge(S.sem, v)
        E.waited[id(S)] = v

    def _deps(self, E, reads, writes, is_dma):
        for b in reads:
            if b.w is not None:
                S, v = b.w
                self._wait(E, S, v)
        for b in writes:
            if b.w is not None:
                S, v = b.w
                if is_dma or S is not E.s or E.name != "pe":
                    self._wait(E, S, v)
            for S, v in b.r.values():
                self._wait(E, S, v)

    def _mark(self, tok, reads, writes):
        S = tok[0]
        for b in reads:
            b.r[id(S)] = tok
        for b in writes:
            b.w = tok
            b.r = {}

    def op(self, E, fn, reads=(), writes=()):
        self._deps(E, reads, writes, False)
        ins = fn(E.eng)
        E.s.cnt += 1
        ins.then_inc(E.s.sem, 1)
        self._mark((E.s, E.s.cnt), reads, writes)
        return ins

    def dma(self, E, out, in_, reads=(), writes=()):
        self._deps(E, reads, writes, True)
        kind = "sw" if E.name == "pool" else "hw"
        pool = self.dsems_sw if kind == "sw" else self.dsems_hw
        d = pool[self.dnext[kind]]
        self.dnext[kind] = (self.dnext[kind] + 1) % len(pool)
        self._wait(E, d, d.cnt)
        ins = E.eng.dma_start(out=out, in_=in_)
        d.cnt += 16
        ins.then_inc(d.sem, 16)
        self._mark((d, d.cnt), reads, writes)

    def coll(self, kind, ins, outs, groups):
        ins_ = self.pool.eng.collective_compute(kind, ALU.bypass, replica_groups=groups, ins=ins, outs=outs)
        self.csem.cnt += 1
        ins_.then_inc(self.csem.sem, 1)

    def barrier(self):
        sems = [e.s for e in self.engs] + self.dsems + [self.csem]
        for E in self.engs:
            for S in sems:
                self._wait(E, S, S.cnt)

    def final_wait(self):
        for S in self.dsems:
            self._wait(self.sp, S, S.cnt)
        for e in self.engs:
            self._wait(self.sp, e.s, e.s.cnt)

    def sb(self, st, shape, dt, name=None):
        self.uid += 1
        t = st.enter_context(self.nc.sbuf_tensor("%s_%d" % (name or "t", self.uid), list(shape), dt))
        return t, Buf(name or "t")

    def ring(self, st, n, shape, dt, name="r"):
        return Ring([self.sb(st, shape, dt, name + str(i)) for i in range(n)])


class Ring:
    def __init__(self, items):
        self.items = items
        self.i = 0

    def next(self):
        it = self.items[self.i]
        self.i = (self.i + 1) % len(self.items)
        return it


TOKB = 8448
NTB = 66
NPD = 27
TOKL = 4352
NTL = 34


def half_tiles(s, with_ctx=True):
    t = list(range(32))
    if with_ctx:
        t += [32, 33]
    return t


def half_blocks(s, with_ctx=True):
    b = [(i * 512, 512) for i in range(8)]
    if with_ctx:
        b.append((HALF, 256))
    return b


def loc(s, t0):
    return t0


def lslot(idx):
    if 0 <= idx <= 31:
        return idx
    if idx >= 32:
        assert idx - 32 < 2
        return idx
    assert idx >= -2
    return 36 + idx


def l_offsets(it, mixer):
    if mixer == "C":
        return [-1, 0, 1]
    if it == 0:
        return [-2, -1, 0, 1, 2, 3]
    if it == 31:
        return [-3, -2, -1, 0, 1, 2]
    return [-2, -1, 0, 1, 2]


def l_pid(it, o, mixer):
    if mixer == "C":
        if it == 0 and o == -1:
            return 3
        if it == 31 and o == 1:
            return 4
        return o + 1
    if it == 0:
        return 5 + (o + 2)
    if it == 1:
        return 11 + (o + 2)
    if it == 30:
        return 16 + (o + 2)
    if it == 31:
        return 21 + (o + 3)
    return o + 2


def d_keytiles(g):
    if g <= 1:
        return [0, 1, 2, 3]
    if g >= 62:
        return [60, 61, 62, 63]
    return [g - 2, g - 1, g, g + 1, g + 2]


def d_pid(g, kt):
    if g == 0:
        return 5 + kt
    if g == 1:
        return 9 + kt
    if g == 62:
        return 13 + (kt - 60)
    if g == 63:
        return 17 + (kt - 60)
    return kt - g + 2


class Prog:
    def __init__(self, layers=(0, 1), debug=None, limit=None, n_cores=8):
        self.n_cores = n_cores
        self.layers = layers
        self.debug = debug or {}
        self.limit = limit or {}
        self.nc = bass.Bass("TRN2", target_bir_lowering=False)
        self.ins = {}
        self.outs = {}

    def din(self, name, shape, dt=F32):
        t = self.nc.dram_tensor(name, list(shape), dt, kind="ExternalInput").ap()
        self.ins[name] = (tuple(shape), dt)
        return t

    def dscr(self, name, shape, dt, out=False):
        kind = "ExternalOutput" if (out or name in self.debug) else "Internal"
        t = self.nc.dram_tensor(name, list(shape), dt, kind=kind).ap()
        if kind == "ExternalOutput":
            self.outs[name] = (tuple(shape), dt)
        return t

    def build(self):
        nc = self.nc
        with contextlib.ExitStack() as st, nc.allow_low_precision("bf16 matmul per problem tolerance"):
            B = self.B = Builder(nc, st)
            self.gst = st
            self.xin = self.din("xin", [TOKL, D])
            self.cvec = self.din("cvec", [128, 16])
            self.consts_in()
            self.L = [self.layer_inputs(l) for l in range(2)]
            self.xcur = self.dscr("xcur", [TOKL, D], F32)
            self.qT_d = self.dscr("qT_d", [D, TOKL], BF16)
            self.ksend = [[self.dscr("ksend%d_%d" % (i, j), [256, HALF], BF16) for j in range(3)] for i in range(2)]
            self.vsend = [[self.dscr("vsend%d_%d" % (i, j), [1024, 768], BF16) for j in range(4)] for i in range(2)]
            self.kall = [[self.dscr("kall%d_%d" % (i, j), [512, HALF], BF16) for j in range(3)] for i in range(2)]
            self.vall = [[self.dscr("vall%d_%d" % (i, j), [2048, 768], BF16) for j in range(4)] for i in range(2)]
            self.kctx = self.dscr("kctx", [768, CTX], BF16)
            self.vctx = self.dscr("vctx", [CTX, 768], BF16)
            self.gT_d = self.dscr("gT_d", [4096, TOKL], BF16)
            self.br_d = self.dscr("br_d", [TOKL, D], BF16)
            self.hT_d = self.dscr("hT_d", [D, TOKL], BF16)
            self.y = self.dscr("y", [HALF, D], F32, out=True)
            self.banks = []
            for i in range(8):
                t = st.enter_context(nc.psum_tensor("ps%d" % i, [128, 512], F32))
                self.banks.append((t, Buf("ps%d" % i)))
            self.pring = Ring(self.banks)
            self.consts_sb()
            for l in self.layers:
                self.layer(l)
            B.barrier()
            B.final_wait()
        return nc

    def consts_in(self):
        self.c_ident = self.din("ident", [128, 128])
        self.c_blk64 = self.din("blk64", [128, 128])
        self.c_blk32 = self.din("blk32", [128, 128])
        self.c_rot64 = self.din("rot64", [128, 128])
        self.c_rot32 = self.din("rot32", [128, 128])
        self.c_cos64 = self.din("cos64", [128, HALF])
        self.c_sin64 = self.din("sin64", [128, HALF])
        self.c_cos32 = self.din("cos32", [128, HALF])
        self.c_sin32 = self.din("sin32", [128, HALF])
        self.c_sel = self.din("sel", [128, 2])
        self.c_cmask = self.din("cmask", [128, 5 * 128])
        self.c_dmask = self.din("dmask", [128, NPD * 128])

    def layer_inputs(self, l):
        s = "_%d" % l
        L = {}
        L["wada"] = self.din("wada" + s, [D, 6 * D])
        L["bada"] = self.din("bada" + s, [128, 48])
        L["badarow"] = self.din("badarow" + s, [1, 6 * D])
        L["n1g"] = self.din("n1g" + s, [128, 8])
        L["n2g"] = self.din("n2g" + s, [128, 8])
        L["win"] = self.din("win" + s, [D, 6656])
        L["bgate"] = self.din("bgate" + s, [128, 32])
        L["qkg"] = self.din("qkg" + s, [128, 14])
        L["lamv"] = self.din("lamv" + s, [128, 128])
        L["subg"] = self.din("subg" + s, [128, 64])
        L["sinkb"] = self.din("sinkb" + s, [128, 4])
        L["dtab"] = self.din("dtab" + s, [128, NPD * 4 * 128])
        L["wbr"] = self.din("wbr" + s, [D, D])
        L["wout"] = self.din("wout" + s, [D, D])
        L["rtw"] = self.din("rtw" + s, [D, NE])
        L["rtb"] = self.din("rtb" + s, [128, NE])
        L["wgu"] = self.din("wgu" + s, [NE * D, 2 * D])
        L["bgu"] = self.din("bgu" + s, [128, NE * 16])
        L["wdn"] = self.din("wdn" + s, [NE * D, D])
        L["bdn"] = self.din("bdn" + s, [NE, D])
        return L

    def consts_sb(self):
        B, st = self.B, self.gst
        self.ident, self.ident_b = B.sb(st, [128, 128], F32, "ident")
        B.dma(B.sp, self.ident[:], self.c_ident[:, :], writes=[self.ident_b])
        self.identb, self.identb_b = B.sb(st, [128, 128], BF16, "identb")
        B.dma(B.pool, self.identb[:], self.c_ident[:, :], writes=[self.identb_b])
        self.cb = {}
        for nm, src in (("blk64", self.c_blk64), ("blk32", self.c_blk32), ("rot64", self.c_rot64), ("rot32", self.c_rot32)):
            t, b = B.sb(st, [128, 128], BF16, nm)
            B.dma(B.pool, t[:], src[:, :], writes=[b])
            self.cb[nm] = (t, b)
        self.ones1, self.ones1_b = B.sb(st, [1, 128], F32, "ones1")
        B.op(B.dve, lambda e: e.memset(self.ones1[:], 1.0), writes=[self.ones1_b])

    def layer(self, l):
        self.l = l
        self.Lw = self.L[l]
        self.need_ctx = (l == 0)
        B = self.B
        stop = self.limit.get("stop")
        with contextlib.ExitStack() as lst:
            self.lst = lst
            self.phase_mod()
            for s in (0,):
                with contextlib.ExitStack() as pst:
                    self.hT, self.hT_b = B.sb(pst, [128, 8, 4352], BF16, "hT")
                    self.phase_norm(1, s)
                    self.phase_proj(s)
            self.phase_gather()
            if stop == "proj":
                return
            with contextlib.ExitStack() as ast_:
                self.ast = ast_
                self.phase_attn_setup()
                for s in (0,):
                    for mixer in self.limit.get("mixers", "ABCD"):
                        if mixer in "AB":
                            self.attn_global(s, mixer)
                        else:
                            self.attn_local(s, mixer)
                    if stop == "attn":
                        continue
                    self.phase_merge(s)
                    if stop == "merge":
                        continue
                    self.phase_norm(2, s)
            if stop in ("attn", "merge", "norm2"):
                return
            self.phase_moe()

    def phase_mod(self):
        B, nc, Lw = self.B, self.nc, self.Lw
        lst = self.lst
        self.modT, self.modT_b = B.sb(lst, [128, 48, 2], F32, "modT")
        self.G, self.G_b = B.sb(lst, [128, 2, 2, 8], F32, "G")
        self.rw, self.rw_b = B.sb(lst, [128, NTL, NE], F32, "rw")
        self.gbc = {}
        for w in (1, 2):
            for j in (0, 1):
                self.gbc[(w, j)] = B.sb(lst, [128, D], F32, "gbc%d%d" % (w, j))
        with contextlib.ExitStack() as st:
            cv, cv_b = B.sb(st, [128, 16], F32, "cv")
            sl, sl_b = B.sb(st, [128, 16], F32, "silu")
            rep, rep_b = B.sb(st, [128, 16, 128], F32, "rep")
            bada, bada_b = B.sb(st, [128, 48], F32, "bada")
            brow, brow_b = B.sb(st, [1, 6 * D], F32, "brow")
            n1g, n1g_b = B.sb(st, [128, 8], F32, "n1g")
            n2g, n2g_b = B.sb(st, [128, 8], F32, "n2g")
            tmp, tmp_b = B.sb(st, [128, 8], F32, "tmpm")
            war = B.ring(st, 2, [128, 8, 512], F32, "wa")
            B.dma(B.sp, cv[:], self.cvec[:, :], writes=[cv_b])
            B.dma(B.sp, bada[:], Lw["bada"][:, :], writes=[bada_b])
            B.dma(B.sp, brow[:], Lw["badarow"][:, :], writes=[brow_b])
            B.dma(B.sp, n1g[:], Lw["n1g"][:, :], writes=[n1g_b])
            B.dma(B.sp, n2g[:], Lw["n2g"][:, :], writes=[n2g_b])
            B.op(B.act, lambda e: e.activation(out=sl[:], in_=cv[:], func=AF.Silu), reads=[cv_b], writes=[sl_b])
            for i in range(16):
                B.op(B.dve, lambda e: e.tensor_copy(out=rep[:, i, :], in_=sl[:, i:i + 1].to_broadcast([128, 128])),
                     reads=[sl_b], writes=[rep_b])
            wsrc = Lw["wada"].rearrange("(k p) n -> p k n", p=128)
            pm, pm_b = self.pring.next()
            for c in range(12):
                wa, wa_b = war.next()
                B.dma(B.sp, wa[:], wsrc[:, :, c * 512:(c + 1) * 512], writes=[wa_b])
                for i in range(4):
                    cc = c * 4 + i
                    for k in range(8):
                        B.op(B.pe, lambda e: e.matmul(pm[:, 2 * cc:2 * cc + 2], lhsT=wa[:, k, i * 128:(i + 1) * 128],
                                                      rhs=sl[:, 2 * k:2 * k + 2], start=(k == 0), stop=(k == 7),
                                                      skip_group_check=True),
                             reads=[wa_b, sl_b], writes=[pm_b])
                if c in (4, 5, 10, 11):
                    w = 1 if c < 6 else 2
                    half = c % 2
                    for j in (0, 1):
                        pb, pb_b = self.pring.next()
                        if pb_b is pm_b:
                            pb, pb_b = self.pring.next()
                        for k in range(8):
                            B.op(B.pe, lambda e: e.matmul(pb[:, :], lhsT=rep[:, 2 * k + j, :], rhs=wa[:, k, :],
                                                          start=(k == 0), stop=False, skip_group_check=True),
                                 reads=[rep_b, wa_b], writes=[pb_b])
                        B.op(B.pe, lambda e: e.matmul(pb[:, :], lhsT=self.ones1[:, :], rhs=brow[0:1, c * 512:(c + 1) * 512],
                                                      start=False, stop=True, skip_group_check=True),
                             reads=[self.ones1_b, brow_b], writes=[pb_b])
                        gt, gb = self.gbc[(w, j)]
                        B.op(B.act, lambda e: e.activation(out=gt[:, half * 512:(half + 1) * 512], in_=pb[:, :], func=AF.Copy),
                             reads=[pb_b], writes=[gb])
            B.op(B.dve, lambda e: e.tensor_tensor(out=self.modT[:], in0=pm[:, 0:96].rearrange("p (c j) -> p c j", j=2),
                                                  in1=bada[:].unsqueeze(2).to_broadcast([128, 48, 2]), op=ALU.add),
                 reads=[pm_b, bada_b], writes=[self.modT_b])
            for w, (ng, ng_b, sc0) in ((0, (n1g, n1g_b, 8)), (1, (n2g, n2g_b, 32))):
                for j in (0, 1):
                    B.op(B.dve, lambda e: e.tensor_scalar(out=tmp[:], in0=self.modT[:, sc0:sc0 + 8, j], scalar1=1.0,
                                                          scalar2=None, op0=ALU.add),
                         reads=[self.modT_b], writes=[tmp_b])
                    B.op(B.dve, lambda e: e.tensor_tensor(out=self.G[:, w, j, :], in0=tmp[:], in1=ng[:], op=ALU.mult),
                         reads=[tmp_b, ng_b], writes=[self.G_b])
            B.barrier()

    def phase_norm(self, which, s):
        B, nc = self.B, self.nc
        l = self.l
        w = which - 1
        sh0 = 0 if which == 1 else 24
        src = self.xin if (l == 0 and which == 1) else self.xcur
        tiles = half_tiles(s, with_ctx=True)
        with contextlib.ExitStack() as st:
            xr = B.ring(st, 2, [128, D], F32, "xt")
            sqr = B.ring(st, 1, [128, D], F32, "sq")
            xsr = B.ring(st, 2, [128, D], F32, "xs")
            ssr = B.ring(st, 2, [128, 4], F32, "ss")
            tmr = B.ring(st, 2, [128, 8, 128], F32, "tm")
            hfr = B.ring(st, 2, [128, 8, 128], F32, "hf")
            hbr = B.ring(st, 2, [128, 8, 128], BF16, "hb")
            if which == 2:
                self.router_setup(st)
            if which == 2 and "moe_tiles" in self.limit:
                tiles = [t for t in tiles if t in self.limit["moe_tiles"]]
            for r in tiles:
                if which == 2 and r >= 32 and not self.need_ctx:
                    continue
                j = 0 if r < 32 else 1
                xt, xt_b = xr.next()
                sq, sq_b = sqr.next()
                xs, xs_b = xsr.next()
                ss, ss_b = ssr.next()
                tm, tm_b = tmr.next()
                hf, hf_b = hfr.next()
                B.dma(B.sp, xt[:], src[r * 128:(r + 1) * 128, :], writes=[xt_b])
                B.op(B.dve, lambda e: e.tensor_tensor(out=sq[:], in0=xt[:], in1=xt[:], op=ALU.mult), reads=[xt_b], writes=[sq_b])
                B.op(B.dve, lambda e: e.reduce_sum(out=ss[:, 0:1], in_=sq[:], axis=AX.X), reads=[sq_b], writes=[ss_b])
                B.op(B.act, lambda e: e.activation(out=ss[:, 1:2], in_=ss[:, 0:1], func=AF.Sqrt, bias=EPS, scale=1.0 / D),
                     reads=[ss_b], writes=[ss_b])
                B.op(B.dve, lambda e: e.reciprocal(out=ss[:, 2:3], in_=ss[:, 1:2]), reads=[ss_b], writes=[ss_b])
                B.op(B.act, lambda e: e.activation(out=xs[:], in_=xt[:], func=AF.Copy, scale=ss[:, 2:3]),
                     reads=[xt_b, ss_b], writes=[xs_b])
                for hb in range(2):
                    pt, pt_b = self.pring.next()
                    for kk in range(4):
                        k = hb * 4 + kk
                        B.op(B.pe, lambda e: e.transpose(out=pt[:, kk * 128:(kk + 1) * 128], in_=xs[:, k * 128:(k + 1) * 128],
                                                         identity=self.ident[:]),
                             reads=[xs_b, self.ident_b], writes=[pt_b])
                    B.op(B.dve, lambda e: e.tensor_tensor(
                        out=tm[:, hb * 4:hb * 4 + 4, :], in0=pt[:, :].rearrange("p (k t) -> p k t", t=128),
                        in1=self.G[:, w, j, hb * 4:hb * 4 + 4].unsqueeze(2).to_broadcast([128, 4, 128]), op=ALU.mult),
                        reads=[pt_b, self.G_b], writes=[tm_b])
                    B.op(B.dve, lambda e: e.tensor_tensor(
                        out=hf[:, hb * 4:hb * 4 + 4, :], in0=tm[:, hb * 4:hb * 4 + 4, :],
                        in1=self.modT[:, sh0 + hb * 4:sh0 + hb * 4 + 4, j].unsqueeze(2).to_broadcast([128, 4, 128]), op=ALU.add),
                        reads=[tm_b, self.modT_b], writes=[hf_b])
                if which == 1:
                    lo = loc(s, r * 128)
                    B.op(B.act, lambda e: e.activation(out=self.hT[:, :, lo:lo + 128], in_=hf[:], func=AF.Copy),
                         reads=[hf_b], writes=[self.hT_b])
                else:
                    hb_, hb_b = hbr.next()
                    B.op(B.act, lambda e: e.activation(out=hb_[:], in_=hf[:], func=AF.Copy), reads=[hf_b], writes=[hb_b])
                    B.dma(B.sp, self.hT_d.rearrange("(k p) t -> p k t", p=128)[:, :, r * 128:(r + 1) * 128], hb_[:], reads=[hb_b])
                    self.router_tile(r, hf, hf_b)
            B.barrier()

    def router_setup(self, st):
        B, Lw = self.B, self.Lw
        self.rtw, self.rtw_b = B.sb(st, [128, 8, NE], F32, "rtw")
        self.rtb, self.rtb_b = B.sb(st, [128, NE], F32, "rtb")
        B.dma(B.sp, self.rtw[:], Lw["rtw"].rearrange("(k p) e -> p k e", p=128), writes=[self.rtw_b])
        B.dma(B.sp, self.rtb[:], Lw["rtb"][:, :], writes=[self.rtb_b])
        self.rt_lg = B.ring(st, 2, [128, NE], F32, "lg")
        self.rt_t8 = B.ring(st, 2, [128, 8], F32, "t8")
        self.rt_sm = B.ring(st, 2, [128, 4], F32, "rsm")
        self.rt_mk = B.ring(st, 2, [128, NE], F32, "mk")
        self.rt_ex = B.ring(st, 2, [128, NE], F32, "ex")

    def router_tile(self, r, hf, hf_b):
        B = self.B
        ps, ps_b = self.pring.next()
        for k in range(8):
            B.op(B.pe, lambda e: e.matmul(ps[:, 0:NE], lhsT=hf[:, k, :], rhs=self.rtw[:, k, :], start=(k == 0), stop=(k == 7)),
                 reads=[hf_b, self.rtw_b], writes=[ps_b])
        lg, lg_b = self.rt_lg.next()
        t8, t8_b = self.rt_t8.next()
        sm, sm_b = self.rt_sm.next()
        mk, mk_b = self.rt_mk.next()
        ex, ex_b = self.rt_ex.next()
        B.op(B.dve, lambda e: e.tensor_tensor(out=lg[:], in0=ps[:, 0:NE], in1=self.rtb[:], op=ALU.add), reads=[ps_b, self.rtb_b], writes=[lg_b])
        B.op(B.dve, lambda e: e.max(out=t8[:], in_=lg[:]), reads=[lg_b], writes=[t8_b])
        B.op(B.dve, lambda e: e.tensor_scalar(out=mk[:], in0=lg[:], scalar1=t8[:, 3:4], scalar2=None, op0=ALU.is_ge),
             reads=[lg_b, t8_b], writes=[mk_b])
        B.op(B.dve, lambda e: e.tensor_scalar(out=sm[:, 0:1], in0=t8[:, 0:1], scalar1=-1.0, scalar2=None, op0=ALU.mult),
             reads=[t8_b], writes=[sm_b])
        B.op(B.act, lambda e: e.activation(out=ex[:], in_=lg[:], func=AF.Exp, bias=sm[:, 0:1], scale=1.0),
             reads=[lg_b, sm_b], writes=[ex_b])
        B.op(B.dve, lambda e: e.tensor_tensor(out=ex[:], in0=ex[:], in1=mk[:], op=ALU.mult), reads=[ex_b, mk_b], writes=[ex_b])
        B.op(B.dve, lambda e: e.reduce_sum(out=sm[:, 1:2], in_=ex[:], axis=AX.X), reads=[ex_b], writes=[sm_b])
        B.op(B.dve, lambda e: e.reciprocal(out=sm[:, 2:3], in_=sm[:, 1:2]), reads=[sm_b], writes=[sm_b])
        B.op(B.dve, lambda e: e.tensor_scalar(out=self.rw[:, r, :], in0=ex[:], scalar1=sm[:, 2:3], scalar2=None, op0=ALU.mult),
             reads=[ex_b, sm_b], writes=[self.rw_b])

    def phase_proj(self, s):
        B, nc, Lw = self.B, self.nc, self.Lw
        wsrc = Lw["win"].rearrange("(k p) n -> p k n", p=128)
        with contextlib.ExitStack() as st:
            wr = B.ring(st, 2, [128, 8, 512], BF16, "wp")
            bg, bg_b = B.sb(st, [128, 32], F32, "bg")
            qkg, qkg_b = B.sb(st, [128, 14], F32, "qkg")
            B.dma(B.sp, bg[:], Lw["bgate"][:, :], writes=[bg_b])
            B.dma(B.sp, qkg[:], Lw["qkg"][:, :], writes=[qkg_b])
            cs = {}
            tabsrc = (("cos64", self.c_cos64), ("sin64", self.c_sin64), ("cos32", self.c_cos32), ("sin32", self.c_sin32))
            for nm, srcap in tabsrc:
                cs[nm] = B.ring(st, 2, [128, 512], F32, nm)
            sqr = B.ring(st, 2, [128, 512], BF16, "sqq")
            rsr = B.ring(st, 2, [128, 512], F32, "rs")
            qnr = B.ring(st, 2, [128, 512], BF16, "qn")
            t1r = B.ring(st, 2, [128, 512], F32, "t1")
            t2r = B.ring(st, 2, [128, 512], F32, "t2")
            outr = B.ring(st, 3, [128, 512], BF16, "po")
            vr = B.ring(st, 2, [128, 768], BF16, "vsb")
            pieces = [(0, 512, "q", 0), (512, 512, "q", 4), (1024, 512, "k", 8), (1536, 256, "k", 12)]
            pieces += [(2560 + 512 * i, 512, "g", 4 * i) for i in range(8)]
            gsz = [64, 64, 32, 32, 64, 64, 64, 64, 64, 32, 32, 64, 64, 64]
            rope = [1, 1, 1, 1, 1, 1, 0, 0, 1, 1, 1, 1, 0, 0]
            for (c0, ncol, kind, ch0) in pieces:
                wp, wp_b = wr.next()
                B.dma(B.pool, wp[:, :, 0:ncol], wsrc[:, :, c0:c0 + ncol], writes=[wp_b])
                for (t0, n) in half_blocks(s):
                    latent = t0 < HALF
                    lo = loc(s, t0)
                    if kind in "gq" and (not latent) and not self.need_ctx:
                        continue
                    ctab = {}
                    if kind != "g" and latent and c0 != 1536:
                        for nm, srcap in tabsrc:
                            t, b = cs[nm].next()
                            B.dma(B.sp, t[:, 0:n], srcap[:, t0:t0 + n], writes=[b])
                            ctab[nm] = (t, b)
                    for i in range(ncol // 128):
                        ps, ps_b = self.pring.next()
                        for k in range(8):
                            B.op(B.pe, lambda e: e.matmul(ps[:, 0:n], lhsT=wp[:, k, i * 128:(i + 1) * 128], rhs=self.hT[:, k, lo:lo + n],
                                                          start=(k == 0), stop=(k == 7)),
                                 reads=[wp_b, self.hT_b], writes=[ps_b])
                        po, po_b = outr.next()
                        if kind == "g":
                            gc = ch0 + i
                            B.op(B.act, lambda e: e.activation(out=po[:, 0:n], in_=ps[:, 0:n], func=AF.Sigmoid, bias=bg[:, gc:gc + 1]),
                                 reads=[ps_b, bg_b], writes=[po_b])
                            B.dma(B.sp, self.gT_d[gc * 128:(gc + 1) * 128, t0:t0 + n], po[:, 0:n], reads=[po_b])
                            continue
                        ci = ch0 + i
                        gs = gsz[ci]
                        sq, sq_b = sqr.next()
                        rs, rs_b = rsr.next()
                        qn, qn_b = qnr.next()
                        B.op(B.act, lambda e: e.activation(out=sq[:, 0:n], in_=ps[:, 0:n], func=AF.Square), reads=[ps_b], writes=[sq_b])
                        blk, blk_b = self.cb["blk%d" % gs]
                        p2, p2_b = self.pring.next()
                        B.op(B.pe, lambda e: e.matmul(p2[:, 0:n], lhsT=blk[:], rhs=sq[:, 0:n], start=True, stop=True),
                             reads=[blk_b, sq_b], writes=[p2_b])
                        B.op(B.act, lambda e: e.activation(out=rs[:, 0:n], in_=p2[:, 0:n], func=AF.Sqrt, bias=EPS, scale=1.0),
                             reads=[p2_b], writes=[rs_b])
                        B.op(B.dve, lambda e: e.reciprocal(out=rs[:, 0:n], in_=rs[:, 0:n]), reads=[rs_b], writes=[rs_b])
                        dorope = rope[ci] and latent
                        dst = qn if dorope else po
                        dst_b = qn_b if dorope else po_b
                        B.op(B.dve, lambda e: e.scalar_tensor_tensor(out=dst[:, 0:n], in0=ps[:, 0:n], scalar=qkg[:, ci:ci + 1], in1=rs[:, 0:n],
                                                                     op0=ALU.mult, op1=ALU.mult),
                             reads=[ps_b, qkg_b, rs_b], writes=[dst_b])
                        if dorope:
                            rot, rot_b = self.cb["rot%d" % gs]
                            p3, p3_b = self.pring.next()
                            B.op(B.pe, lambda e: e.matmul(p3[:, 0:n], lhsT=rot[:], rhs=qn[:, 0:n], start=True, stop=True),
                                 reads=[rot_b, qn_b], writes=[p3_b])
                            ct, ct_b = ctab["cos%d" % gs]
                            sn, sn_b = ctab["sin%d" % gs]
                            t1, t1_b = t1r.next()
                            t2, t2_b = t2r.next()
                            B.op(B.pool, lambda e: e.tensor_tensor(out=t1[:, 0:n], in0=qn[:, 0:n], in1=ct[:, 0:n], op=ALU.mult),
                                 reads=[qn_b, ct_b], writes=[t1_b])
                            B.op(B.dve, lambda e: e.tensor_tensor(out=t2[:, 0:n], in0=p3[:, 0:n], in1=sn[:, 0:n], op=ALU.mult),
                                 reads=[p3_b, sn_b], writes=[t2_b])
                            B.op(B.dve, lambda e: e.tensor_tensor(out=po[:, 0:n], in0=t1[:, 0:n], in1=t2[:, 0:n], op=ALU.add),
                                 reads=[t1_b, t2_b], writes=[po_b])
                        if kind == "q":
                            B.dma(B.sp, self.qT_d[ci * 128:(ci + 1) * 128, t0:t0 + n], po[:, 0:n], reads=[po_b])
                        else:
                            kc = ci - 8
                            if latent:
                                B.dma(B.sp, self.k_own(kc)[:, t0:t0 + n], po[:, 0:n], reads=[po_b])
                            else:
                                B.dma(B.sp, self.kctx[kc * 128:(kc + 1) * 128, 0:n], po[:, 0:n], reads=[po_b])
            wv, wv_b = B.sb(st, [128, 8, 768], BF16, "wv")
            B.dma(B.pool, wv[:], wsrc[:, :, 1792:2560], writes=[wv_b])
            for r in half_tiles(s):
                lo = loc(s, r * 128)
                pa, pa_b = self.pring.next()
                pb, pb_b = self.pring.next()
                for k in range(8):
                    B.op(B.pe, lambda e: e.matmul(pa[:, :], lhsT=self.hT[:, k, lo:lo + 128], rhs=wv[:, k, 0:512],
                                                  start=(k == 0), stop=(k == 7)), reads=[self.hT_b, wv_b], writes=[pa_b])
                for k in range(8):
                    B.op(B.pe, lambda e: e.matmul(pb[:, 0:256], lhsT=self.hT[:, k, lo:lo + 128], rhs=wv[:, k, 512:768],
                                                  start=(k == 0), stop=(k == 7)), reads=[self.hT_b, wv_b], writes=[pb_b])
                vs, vs_b = vr.next()
                B.op(B.act, lambda e: e.activation(out=vs[:, 0:512], in_=pa[:, :], func=AF.Copy), reads=[pa_b], writes=[vs_b])
                B.op(B.dve, lambda e: e.tensor_copy(out=vs[:, 512:768], in_=pb[:, 0:256]), reads=[pb_b], writes=[vs_b])
                if r < 32:
                    B.dma(B.sp, self.vsend[self.l][r // 8][(r % 8) * 128:(r % 8 + 1) * 128, :], vs[:], reads=[vs_b])
                else:
                    B.dma(B.sp, self.vctx[(r - 32) * 128:(r - 31) * 128, :], vs[:], reads=[vs_b])
            B.barrier()

    def phase_attn_setup(self):
        B, Lw = self.B, self.Lw
        lst = self.ast
        lam_init = 0.8 - 0.6 * math.exp(-0.3 * self.l)
        self.lam_init = lam_init
        self.neglam, self.neglam_b = B.sb(lst, [128, 4], F32, "neglam")
        self.esink, self.esink_b = B.sb(lst, [128, 4], F32, "esink")
        self.subg, self.subg_b = B.sb(lst, [128, 64], F32, "subg")
        self.dE, self.dE_b = B.sb(lst, [128, NPD * 4, 128], BF16, "dE")
        self.cM, self.cM_b = B.sb(lst, [128, 5, 128], BF16, "cM")
        with contextlib.ExitStack() as st:
            lv, lv_b = B.sb(st, [128, 4, 32], F32, "lv")
            pr, pr_b = B.sb(st, [128, 2, 32], F32, "pr")
            sm, sm_b = B.sb(st, [128, 4], F32, "lsm")
            B.dma(B.sp, lv[:], Lw["lamv"].rearrange("p (a b) -> p a b", b=32), writes=[lv_b])
            B.dma(B.sp, self.subg[:], Lw["subg"][:, :], writes=[self.subg_b])
            B.dma(B.sp, self.esink[:], Lw["sinkb"][:, :], writes=[self.esink_b])
            B.op(B.act, lambda e: e.activation(out=self.esink[:], in_=self.esink[:], func=AF.Exp), reads=[self.esink_b], writes=[self.esink_b])
            B.op(B.dve, lambda e: e.tensor_scalar(out=self.subg[:], in0=self.subg[:], scalar1=float(1.0 - lam_init), scalar2=None, op0=ALU.mult),
                 reads=[self.subg_b], writes=[self.subg_b])
            B.op(B.dve, lambda e: e.tensor_tensor(out=pr[:, 0, :], in0=lv[:, 0, :], in1=lv[:, 1, :], op=ALU.mult), reads=[lv_b], writes=[pr_b])
            B.op(B.dve, lambda e: e.tensor_tensor(out=pr[:, 1, :], in0=lv[:, 2, :], in1=lv[:, 3, :], op=ALU.mult), reads=[lv_b], writes=[pr_b])
            B.op(B.dve, lambda e: e.reduce_sum(out=sm[:, 0:2], in_=pr[:], axis=AX.X), reads=[pr_b], writes=[sm_b])
            B.op(B.act, lambda e: e.activation(out=sm[:, 2:4], in_=sm[:, 0:2], func=AF.Exp), reads=[sm_b], writes=[sm_b])
            B.op(B.dve, lambda e: e.tensor_tensor(out=sm[:, 0:1], in0=sm[:, 3:4], in1=sm[:, 2:3], op=ALU.subtract), reads=[sm_b], writes=[sm_b])
            B.op(B.dve, lambda e: e.tensor_scalar(out=self.neglam[:, 0:1], in0=sm[:, 0:1], scalar1=float(-lam_init), scalar2=None, op0=ALU.add),
                 reads=[sm_b], writes=[self.neglam_b])
            B.dma(B.pool, self.cM[:], self.c_cmask.rearrange("p (a b) -> p a b", b=128), writes=[self.cM_b])
            dm, dm_b = B.sb(st, [128, NPD, 128], F32, "dm")
            B.dma(B.sp, dm[:], self.c_dmask.rearrange("p (a b) -> p a b", b=128), writes=[dm_b])
            dtr = B.ring(st, 2, [128, 4, 128], F32, "dt")
            dsrc = Lw["dtab"].rearrange("p (a h b) -> p a h b", h=4, b=128)
            for pid in range(NPD):
                dt_, dt_b = dtr.next()
                B.dma(B.sp, dt_[:], dsrc[:, pid, :, :], writes=[dt_b])
                B.op(B.act, lambda e: e.activation(out=dt_[:], in_=dt_[:], func=AF.Exp), reads=[dt_b], writes=[dt_b])
                B.op(B.dve, lambda e: e.tensor_tensor(out=self.dE[:, pid * 4:(pid + 1) * 4, :], in0=dt_[:],
                                                      in1=dm[:, pid:pid + 1, :].to_broadcast([128, 4, 128]), op=ALU.mult),
                     reads=[dt_b, dm_b], writes=[self.dE_b])
            B.barrier()

    def k_own(self, c):
        return self.ksend[self.l][c // 2][(c % 2) * 128:(c % 2 + 1) * 128, :]

    def k_all(self, r, c):
        return self.kall[self.l][c // 2][r * 256 + (c % 2) * 128:r * 256 + (c % 2 + 1) * 128, :]

    def phase_gather(self):
        B = self.B
        l = self.l
        groups = [[2 * i, 2 * i + 1] for i in range(self.n_cores // 2)]
        B.barrier()
        for j in range(3):
            B.coll("AllGather", [self.ksend[l][j].opt()], [self.kall[l][j].opt()], groups)
        for j in range(4):
            B.coll("AllGather", [self.vsend[l][j].opt()], [self.vall[l][j].opt()], groups)
        B.barrier()

    def load_kv(self, st, mixer):
        B = self.B
        l = self.l
        kc0 = {"A": 0, "B": 1}[mixer]
        vc0 = {"A": 0, "B": 128}[mixer]
        nvh = {"A": 2, "B": 4}[mixer]
        nk = 2 if mixer == "A" else 3
        kT, kT_b = B.sb(st, [128, nk, TOKB], BF16, "kT" + mixer)
        srcs = [((lambda c, r=r: self.k_all(r, c)), r * HALF, HALF) for r in range(2)]
        srcs += [((lambda c: self.kctx[c * 128:(c + 1) * 128, :]), SEQ, CTX)]
        for (srcf, c0, n) in srcs:
            if mixer == "A":
                src = srcf(kc0)
                for kv in range(2):
                    for hf in range(2):
                        B.dma(B.sp, kT[hf * 64:(hf + 1) * 64, kv, c0:c0 + n], src[kv * 64:(kv + 1) * 64, :], writes=[kT_b])
            else:
                for c in range(2):
                    src = srcf(kc0 + c)
                    B.dma(B.sp, kT[:, c, c0:c0 + n], src[:, :], writes=[kT_b])
                    B.dma(B.sp, kT[32 * c:32 * c + 32, 2, c0:c0 + n], src[96:128, :], writes=[kT_b])
        va, va_b = B.sb(st, [128, NTB, nvh, 65], BF16, "va" + mixer)
        with contextlib.ExitStack() as st2:
            vs, vs_b = B.sb(st2, [128, NTB, nvh * 64], BF16, "vstage")
            for r in range(2):
                for j in range(4):
                    vsrc = self.vall[l][j].rearrange("(t p) c -> p t c", p=128)
                    B.dma(B.sp, vs[:, r * 32 + j * 8:r * 32 + j * 8 + 8, :], vsrc[:, r * 8:(r + 1) * 8, vc0:vc0 + nvh * 64], writes=[vs_b])
            B.dma(B.sp, vs[:, 64:66, :], self.vctx.rearrange("(t p) c -> p t c", p=128)[:, :, vc0:vc0 + nvh * 64], writes=[vs_b])
            B.op(B.pool, lambda e: e.memset(va[:, :, :, 64:65], 1.0), writes=[va_b])
            B.op(B.dve, lambda e: e.tensor_copy(out=va[:, :, :, 0:64], in_=vs[:].rearrange("p t (h d) -> p t h d", d=64)),
                 reads=[vs_b], writes=[va_b])
            B.barrier()
        return kT, kT_b, va, va_b

    def load_kv_local(self, st, mixer):
        B = self.B
        l = self.l
        kc0 = {"C": 3, "D": 4}[mixer]
        vc0 = {"C": 384, "D": 512}[mixer]
        nvh = {"C": 2, "D": 4}[mixer]
        NS = 38
        kT, kT_b = B.sb(st, [128, 2, NS * 128], BF16, "kT" + mixer)
        va, va_b = B.sb(st, [128, NS, nvh, 65], BF16, "va" + mixer)
        with contextlib.ExitStack() as st2:
            kO, kO_b = B.sb(st2, [128, 2, 2, 512], BF16, "kO")
            tk, tk_b = B.sb(st2, [128, 2, 512], F32, "tk")
            vs, vs_b = B.sb(st2, [128, NS, nvh * 64], BF16, "vstage")
            vO, vO_b = B.sb(st2, [128, 2, 4, nvh * 64], BF16, "vO")
            tv, tv_b = B.sb(st2, [128, 4, nvh * 64], F32, "tv")
            sel, sel_b = B.sb(st2, [128, 2], F32, "sel")
            B.dma(B.sp, sel[:], self.c_sel[:, :], writes=[sel_b])

            def kload(dst, dst_b, cols, srcf, scols):
                if mixer == "C":
                    src = srcf(kc0)
                    for kv in range(2):
                        for hf in range(2):
                            B.dma(B.sp, dst(hf * 64, (hf + 1) * 64, kv, cols), src[kv * 64:(kv + 1) * 64, scols[0]:scols[1]], writes=[dst_b])
                else:
                    for c in range(2):
                        src = srcf(kc0 + c)
                        B.dma(B.sp, dst(0, 128, c, cols), src[:, scols[0]:scols[1]], writes=[dst_b])

            kload(lambda p0, p1, c, cols: kT[p0:p1, c, cols[0]:cols[1]], kT_b, (0, HALF), self.k_own, (0, HALF))
            kload(lambda p0, p1, c, cols: kT[p0:p1, c, cols[0]:cols[1]], kT_b, (36 * 128, 38 * 128), (lambda c: self.kctx[c * 128:(c + 1) * 128, :]), (0, CTX))
            for r in range(2):
                srcf = (lambda c, r=r: self.k_all(r, c))
                kload(lambda p0, p1, c, cols: kO[p0:p1, c, r, cols[0]:cols[1]], kO_b, (0, 256), srcf, (0, 256))
                kload(lambda p0, p1, c, cols: kO[p0:p1, c, r, cols[0]:cols[1]], kO_b, (256, 512), srcf, (HALF - 256, HALF))
            B.op(B.dve, lambda e: e.tensor_scalar(out=tk[:], in0=kO[:, :, 0, :], scalar1=sel[:, 0:1], scalar2=None, op0=ALU.mult),
                 reads=[kO_b, sel_b], writes=[tk_b])
            B.op(B.dve, lambda e: e.scalar_tensor_tensor(out=kT[:, :, 32 * 128:36 * 128], in0=kO[:, :, 1, :], scalar=sel[:, 1:2], in1=tk[:],
                                                         op0=ALU.mult, op1=ALU.add),
                 reads=[kO_b, sel_b, tk_b], writes=[kT_b])
            for j in range(4):
                B.dma(B.sp, vs[:, j * 8:(j + 1) * 8, :], self.vsend[l][j].rearrange("(t p) c -> p t c", p=128)[:, :, vc0:vc0 + nvh * 64], writes=[vs_b])
            B.dma(B.sp, vs[:, 36:38, :], self.vctx.rearrange("(t p) c -> p t c", p=128)[:, :, vc0:vc0 + nvh * 64], writes=[vs_b])
            for r in range(2):
                v0 = self.vall[l][0].rearrange("(t p) c -> p t c", p=128)
                v3 = self.vall[l][3].rearrange("(t p) c -> p t c", p=128)
                B.dma(B.sp, vO[:, r, 0:2, :], v0[:, r * 8:r * 8 + 2, vc0:vc0 + nvh * 64], writes=[vO_b])
                B.dma(B.sp, vO[:, r, 2:4, :], v3[:, r * 8 + 6:r * 8 + 8, vc0:vc0 + nvh * 64], writes=[vO_b])
            B.op(B.dve, lambda e: e.tensor_scalar(out=tv[:], in0=vO[:, 0, :, :], scalar1=sel[:, 0:1], scalar2=None, op0=ALU.mult),
                 reads=[vO_b, sel_b], writes=[tv_b])
            B.op(B.dve, lambda e: e.scalar_tensor_tensor(out=vs[:, 32:36, :], in0=vO[:, 1, :, :], scalar=sel[:, 1:2], in1=tv[:],
                                                         op0=ALU.mult, op1=ALU.add),
                 reads=[vO_b, sel_b, tv_b], writes=[vs_b])
            B.op(B.pool, lambda e: e.memset(va[:, :, :, 64:65], 1.0), writes=[va_b])
            B.op(B.dve, lambda e: e.tensor_copy(out=va[:, :, :, 0:64], in_=vs[:].rearrange("p t (h d) -> p t h d", d=64)),
                 reads=[vs_b], writes=[va_b])
            B.barrier()
        return kT, kT_b, va, va_b

    def load_q(self, st, s, mixer):
        B = self.B
        qc0 = {"A": 0, "B": 2, "C": 4, "D": 6}[mixer]
        n = TOKL if self.need_ctx else HALF
        qT, qT_b = B.sb(st, [128, 3 if mixer == "B" else 2, TOKL], BF16, "qT" + mixer)
        for c in range(2):
            B.dma(B.sp, qT[:, c, 0:n], self.qT_d[(qc0 + c) * 128:(qc0 + c + 1) * 128, 0:n], writes=[qT_b])
            if mixer == "B":
                B.dma(B.sp, qT[32 * c:32 * c + 32, 2, 0:n], self.qT_d[(qc0 + c) * 128 + 96:(qc0 + c + 1) * 128, 0:n], writes=[qT_b])
        return qT, qT_b

    def attn_global(self, s, mixer):
        B = self.B
        br = {"A": 0, "B": 1}[mixer]
        scale = 0.125 if mixer == "A" else 32 ** -0.5
        with contextlib.ExitStack() as st:
            kT, kT_b, va, va_b = self.load_kv(st, mixer)
            qT, qT_b = self.load_q(st, s, mixer)
            pr = B.ring(st, 5, [128, 512], BF16, "P")
            osb = B.ring(st, 2, [128, 2, 4, 65], F32, "osb")
            obr = B.ring(st, 2, [128, 4, 256], BF16, "ob")
            rcr = B.ring(st, 2, [128, 2, 4, 1], F32, "rc")
            o0r = B.ring(st, 2, [128, 4, 64], F32, "o0")
            o1r = B.ring(st, 2, [128, 4, 64], F32, "o1")
            sqr = B.ring(st, 2, [128, 4, 64], F32, "osq")
            ssr = B.ring(st, 2, [128, 4, 2], F32, "oss")
            blocks = half_blocks(s, with_ctx=self.need_ctx)
            if "qblocks" in self.limit:
                blocks = [blocks[i] for i in self.limit["qblocks"] if i < len(blocks)]
            for (t0, n) in blocks:
                lo = loc(s, t0)
                nsb = n // 128
                keytiles = list(range(NTB)) if t0 < HALF else [64, 65]
                ob, ob_b = obr.next()
                for h in range(4):
                    nc_ = 1 if mixer == "A" else 2
                    obanks = [self.pring.next() for _ in range(nc_)]
                    items = [(ki, kt, c) for ki, kt in enumerate(keytiles) for c in range(nc_)]
                    DEPTH = 3
                    pend = []

                    def stage1(item):
                        ki, kt, c = item
                        if mixer == "A":
                            pb = 64 * (h % 2)
                            kap = kT[pb:pb + 64, h // 2, kt * 128:(kt + 1) * 128]
                            qap = qT[pb:pb + 64, h // 2, lo:lo + n]
                            vap = va[:, kt, h // 2, :]
                        else:
                            g = (h % 2) * 2 + c
                            slot, pb = (h // 2, 32 * g) if g < 3 else (2, 32 * (h // 2))
                            kap = kT[pb:pb + 32, slot, kt * 128:(kt + 1) * 128]
                            qap = qT[pb:pb + 32, slot, lo:lo + n]
                            vap = va[:, kt, h, :]
                        sp_, sp_b = self.pring.next()
                        while any(sp_b is ob_[1] for ob_ in obanks):
                            sp_, sp_b = self.pring.next()
                        B.op(B.pe, lambda e: e.matmul(sp_[:, 0:n], lhsT=kap, rhs=qap, start=True, stop=True),
                             reads=[kT_b, qT_b], writes=[sp_b])
                        P, P_b = pr.next()
                        B.op(B.act, lambda e: e.activation(out=P[:, 0:n], in_=sp_[:, 0:n], func=AF.Exp, scale=float(scale)),
                             reads=[sp_b], writes=[P_b])
                        return (P, P_b, vap)

                    def stage2(item, st1):
                        ki, kt, c = item
                        P, P_b, vap = st1
                        ot, ot_b = obanks[c]
                        for sb in range(nsb):
                            B.op(B.pe, lambda e: e.matmul(ot[:, sb * 65:(sb + 1) * 65], lhsT=P[:, sb * 128:(sb + 1) * 128], rhs=vap,
                                                          start=(ki == 0 and sb == 0), stop=(ki == len(keytiles) - 1),
                                                          skip_group_check=True),
                                 reads=[P_b, va_b], writes=[ot_b])

                    for i in range(len(items) + DEPTH):
                        if i < len(items):
                            pend.append(stage1(items[i]))
                        if i >= DEPTH:
                            stage2(items[i - DEPTH], pend.pop(0))
                    os_, os_b = osb.next()
                    rc, rc_b = rcr.next()
                    for c in range(nc_):
                        ot, ot_b = obanks[c]
                        B.op(B.act, lambda e: e.activation(out=os_[:, c, 0:nsb, :], in_=ot[:, 0:nsb * 65].rearrange("p (a b) -> p a b", b=65), func=AF.Copy),
                             reads=[ot_b], writes=[os_b])
                    B.op(B.dve, lambda e: e.reciprocal(out=rc[:, 0:nc_, 0:nsb, :], in_=os_[:, 0:nc_, 0:nsb, 64:65]), reads=[os_b], writes=[rc_b])
                    if mixer == "A":
                        B.op(B.dve, lambda e: e.tensor_tensor(out=ob[:, 0:nsb, h * 64:(h + 1) * 64], in0=os_[:, 0, 0:nsb, 0:64],
                                                              in1=rc[:, 0, 0:nsb, :].to_broadcast([128, nsb, 64]), op=ALU.mult),
                             reads=[os_b, rc_b], writes=[ob_b])
                    else:
                        o0, o0_b = o0r.next()
                        o1, o1_b = o1r.next()
                        sq, sq_b = sqr.next()
                        ss, ss_b = ssr.next()
                        B.op(B.dve, lambda e: e.tensor_tensor(out=o0[:, 0:nsb, :], in0=os_[:, 0, 0:nsb, 0:64],
                                                              in1=rc[:, 0, 0:nsb, :].to_broadcast([128, nsb, 64]), op=ALU.mult),
                             reads=[os_b, rc_b], writes=[o0_b])
                        B.op(B.dve, lambda e: e.tensor_tensor(out=o1[:, 0:nsb, :], in0=os_[:, 1, 0:nsb, 0:64],
                                                              in1=rc[:, 1, 0:nsb, :].to_broadcast([128, nsb, 64]), op=ALU.mult),
                             reads=[os_b, rc_b], writes=[o1_b])
                        B.op(B.dve, lambda e: e.scalar_tensor_tensor(out=o0[:, 0:nsb, :], in0=o1[:, 0:nsb, :], scalar=self.neglam[:, 0:1],
                                                                     in1=o0[:, 0:nsb, :], op0=ALU.mult, op1=ALU.add),
                             reads=[o0_b, o1_b, self.neglam_b], writes=[o0_b])
                        B.op(B.dve, lambda e: e.tensor_tensor(out=sq[:, 0:nsb, :], in0=o0[:, 0:nsb, :], in1=o0[:, 0:nsb, :], op=ALU.mult),
                             reads=[o0_b], writes=[sq_b])
                        B.op(B.dve, lambda e: e.reduce_sum(out=ss[:, 0:nsb, 0:1], in_=sq[:, 0:nsb, :], axis=AX.X), reads=[sq_b], writes=[ss_b])
                        B.op(B.act, lambda e: e.activation(out=ss[:, 0:nsb, 1:2], in_=ss[:, 0:nsb, 0:1], func=AF.Sqrt, bias=EPS, scale=1.0 / 64),
                             reads=[ss_b], writes=[ss_b])
                        B.op(B.dve, lambda e: e.reciprocal(out=ss[:, 0:nsb, 0:1], in_=ss[:, 0:nsb, 1:2]), reads=[ss_b], writes=[ss_b])
                        B.op(B.dve, lambda e: e.tensor_tensor(out=o0[:, 0:nsb, :], in0=o0[:, 0:nsb, :],
                                                              in1=ss[:, 0:nsb, 0:1].to_broadcast([128, nsb, 64]), op=ALU.mult),
                             reads=[o0_b, ss_b], writes=[o0_b])
                        B.op(B.dve, lambda e: e.tensor_tensor(out=ob[:, 0:nsb, h * 64:(h + 1) * 64], in0=o0[:, 0:nsb, :],
                                                              in1=self.subg[:].unsqueeze(1).to_broadcast([128, nsb, 64]), op=ALU.mult),
                             reads=[o0_b, self.subg_b], writes=[ob_b])
                B.dma(B.sp, self.br_d.rearrange("(t p) c -> p t c", p=128)[:, t0 // 128:t0 // 128 + nsb, br * 256:(br + 1) * 256],
                      ob[:, 0:nsb, :], reads=[ob_b])
            B.barrier()

    def attn_local(self, s, mixer):
        B = self.B
        br = {"C": 2, "D": 3}[mixer]
        scale = 0.125
        with contextlib.ExitStack() as st:
            kT, kT_b, va, va_b = self.load_kv_local(st, mixer)
            qT, qT_b = self.load_q(st, s, mixer)
            pr = B.ring(st, 3, [128, 8, 128], BF16, "PL")
            osb = B.ring(st, 2, [128, 4, 65], F32, "osbl")
            obr = B.ring(st, 2, [128, 256], BF16, "obl")
            rcr = B.ring(st, 2, [128, 4, 1], F32, "rcl")
            tiles = half_tiles(s, with_ctx=self.need_ctx)
            if "ltiles" in self.limit:
                tiles = [t for t in tiles if t in self.limit["ltiles"]]
            for g in tiles:
                lo = loc(s, g * 128)
                if g >= 32:
                    offs = []
                else:
                    offs = l_offsets(g, mixer)
                local = [lslot(g + o) for o in offs]
                kts = local + [36, 37]
                nl = len(local)
                ot, ot_b = self.pring.next()
                def stage1(h):
                    pb = 64 * (h % 2)
                    kcol = h // 2
                    qap = qT[pb:pb + 64, h // 2, lo:lo + 128]
                    sa, sa_b = self.pring.next()
                    while sa_b is ot_b:
                        sa, sa_b = self.pring.next()
                    sb_, sb_b = self.pring.next()
                    while sb_b is ot_b or sb_b is sa_b:
                        sb_, sb_b = self.pring.next()
                    P, P_b = pr.next()
                    for j, kt in enumerate(kts):
                        bank, bank_b = (sa, sa_b) if j < 4 else (sb_, sb_b)
                        jj = j % 4
                        kap = kT[pb:pb + 64, kcol, kt * 128:(kt + 1) * 128]
                        B.op(B.pe, lambda e: e.matmul(bank[:, jj * 128:(jj + 1) * 128], lhsT=kap, rhs=qap, start=True, stop=True,
                                                      skip_group_check=True),
                             reads=[kT_b, qT_b], writes=[bank_b])
                    n1 = min(4, len(kts))
                    B.op(B.act, lambda e: e.activation(out=P[:, 0:n1, :], in_=sa[:, 0:n1 * 128].rearrange("p (a b) -> p a b", b=128),
                                                       func=AF.Exp, scale=float(scale)), reads=[sa_b], writes=[P_b])
                    if len(kts) > 4:
                        n2 = len(kts) - 4
                        B.op(B.act, lambda e: e.activation(out=P[:, 4:4 + n2, :], in_=sb_[:, 0:n2 * 128].rearrange("p (a b) -> p a b", b=128),
                                                           func=AF.Exp, scale=float(scale)), reads=[sb_b], writes=[P_b])
                    for j, o in enumerate(offs):
                        if mixer == "C":
                            tab = self.cM[:, l_pid(g, o, "C"), :]
                            tab_b = self.cM_b
                        else:
                            tab = self.dE[:, l_pid(g, o, "D") * 4 + h, :]
                            tab_b = self.dE_b
                        B.op(B.dve, lambda e: e.tensor_tensor(out=P[:, j, :], in0=P[:, j, :], in1=tab, op=ALU.mult),
                             reads=[P_b, tab_b], writes=[P_b])
                    return P, P_b

                def stage2(h, st1):
                    P, P_b = st1
                    kv = h // 2 if mixer == "C" else h
                    for j, kt in enumerate(kts):
                        B.op(B.pe, lambda e: e.matmul(ot[:, h * 65:(h + 1) * 65], lhsT=P[:, j, :], rhs=va[:, kt, kv, :],
                                                      start=(j == 0 and h == 0), stop=(j == len(kts) - 1), skip_group_check=True),
                             reads=[P_b, va_b], writes=[ot_b])

                DEPTH = 2
                pend = []
                for i in range(4 + DEPTH):
                    if i < 4:
                        pend.append(stage1(i))
                    if i >= DEPTH:
                        stage2(i - DEPTH, pend.pop(0))
                os_, os_b = osb.next()
                rc, rc_b = rcr.next()
                ob, ob_b = obr.next()
                B.op(B.act, lambda e: e.activation(out=os_[:], in_=ot[:, 0:260].rearrange("p (a b) -> p a b", b=65), func=AF.Copy),
                     reads=[ot_b], writes=[os_b])
                if mixer == "C":
                    B.op(B.dve, lambda e: e.tensor_tensor(out=rc[:], in0=os_[:, :, 64:65], in1=self.esink[:].unsqueeze(2), op=ALU.add),
                         reads=[os_b, self.esink_b], writes=[rc_b])
                    B.op(B.dve, lambda e: e.reciprocal(out=rc[:], in_=rc[:]), reads=[rc_b], writes=[rc_b])
                else:
                    B.op(B.dve, lambda e: e.reciprocal(out=rc[:], in_=os_[:, :, 64:65]), reads=[os_b], writes=[rc_b])
                B.op(B.dve, lambda e: e.tensor_tensor(out=ob[:].rearrange("p (h d) -> p h d", d=64), in0=os_[:, :, 0:64],
                                                      in1=rc[:].to_broadcast([128, 4, 64]), op=ALU.mult),
                     reads=[os_b, rc_b], writes=[ob_b])
                B.dma(B.sp, self.br_d[g * 128:(g + 1) * 128, br * 256:(br + 1) * 256], ob[:], reads=[ob_b])
            B.barrier()
    def phase_merge(self, s):
        B, Lw = self.B, self.Lw
        xsrc = self.xin if self.l == 0 else self.xcur
        with contextlib.ExitStack() as st:
            wbr, wbr_b = B.sb(st, [128, 8, D], BF16, "wbr")
            wout, wout_b = B.sb(st, [128, 8, D], BF16, "wout")
            B.dma(B.pool, wbr[:], Lw["wbr"].rearrange("(k p) n -> p k n", p=128), writes=[wbr_b])
            B.dma(B.pool, wout[:], Lw["wout"].rearrange("(k p) n -> p k n", p=128), writes=[wout_b])
            brr = B.ring(st, 2, [128, D], BF16, "brt")
            brT, brT_b = B.sb(st, [128, 8, 512], BF16, "brT")
            gtr = B.ring(st, 2, [128, 8, 512], BF16, "gt")
            mg, mg_b = B.sb(st, [128, 8, 512], F32, "mg")
            mgb, mgb_b = B.sb(st, [128, 8, 512], BF16, "mgb")
            tpr = B.ring(st, 2, [128, 512], F32, "tp")
            xr = B.ring(st, 2, [128, D], F32, "xm")
            tr = B.ring(st, 2, [128, D], F32, "tmx")
            gsrc = self.gT_d.rearrange("(c p) t -> p c t", p=128)
            mblocks = half_blocks(s, with_ctx=self.need_ctx)
            if "mblocks" in self.limit:
                mblocks = [mblocks[i] for i in self.limit["mblocks"]]
            for (t0, n) in mblocks:
                nsb = n // 128
                j = 0 if t0 < HALF else 1
                for sb in range(nsb):
                    bt, bt_b = brr.next()
                    r = t0 // 128 + sb
                    B.dma(B.sp, bt[:], self.br_d[r * 128:(r + 1) * 128, :], writes=[bt_b])
                    pt, pt_b = self.pring.next()
                    ptb = pt[:, :].bitcast(BF16)
                    for k in range(8):
                        B.op(B.pe, lambda e: e.transpose(out=ptb[:, k * 128:(k + 1) * 128], in_=bt[:, k * 128:(k + 1) * 128], identity=self.identb[:]),
                             reads=[bt_b, self.identb_b], writes=[pt_b])
                    B.op(B.act, lambda e: e.activation(out=brT[:, :, sb * 128:(sb + 1) * 128], in_=ptb.rearrange("p (k t) -> p k t", t=128), func=AF.Copy),
                         reads=[pt_b], writes=[brT_b])
                for nb in range(4):
                    gt, gt_b = gtr.next()
                    B.dma(B.sp, gt[:, :, 0:n], gsrc[:, nb * 8:(nb + 1) * 8, t0:t0 + n], writes=[gt_b])
                    for dc in range(8):
                        ps, ps_b = self.pring.next()
                        for kk in range(2):
                            B.op(B.pe, lambda e: e.matmul(ps[:, 0:n], lhsT=wbr[:, nb * 2 + kk, dc * 128:(dc + 1) * 128], rhs=brT[:, nb * 2 + kk, 0:n],
                                                          start=(kk == 0), stop=(kk == 1)), reads=[wbr_b, brT_b], writes=[ps_b])
                        if nb == 0:
                            B.op(B.dve, lambda e: e.tensor_tensor(out=mg[:, dc, 0:n], in0=ps[:, 0:n], in1=gt[:, dc, 0:n], op=ALU.mult),
                                 reads=[ps_b, gt_b], writes=[mg_b])
                        else:
                            tp, tp_b = tpr.next()
                            B.op(B.dve, lambda e: e.tensor_tensor(out=tp[:, 0:n], in0=ps[:, 0:n], in1=gt[:, dc, 0:n], op=ALU.mult),
                                 reads=[ps_b, gt_b], writes=[tp_b])
                            B.op(B.pool, lambda e: e.tensor_tensor(out=mg[:, dc, 0:n], in0=mg[:, dc, 0:n], in1=tp[:, 0:n], op=ALU.add),
                                 reads=[mg_b, tp_b], writes=[mg_b])
                B.op(B.act, lambda e: e.activation(out=mgb[:, :, 0:n], in_=mg[:, :, 0:n], func=AF.Copy), reads=[mg_b], writes=[mgb_b])
                gt_, g_b = self.gbc[(1, j)]
                for sb in range(nsb):
                    r = t0 // 128 + sb
                    xt, xt_b = xr.next()
                    tm, tm_b = tr.next()
                    B.dma(B.sp, xt[:], xsrc[r * 128:(r + 1) * 128, :], writes=[xt_b])
                    for hh in range(2):
                        ps, ps_b = self.pring.next()
                        for k in range(8):
                            B.op(B.pe, lambda e: e.matmul(ps[:, :], lhsT=mgb[:, k, sb * 128:(sb + 1) * 128], rhs=wout[:, k, hh * 512:(hh + 1) * 512],
                                                          start=(k == 0), stop=(k == 7)), reads=[mgb_b, wout_b], writes=[ps_b])
                        B.op(B.dve, lambda e: e.tensor_tensor(out=tm[:, hh * 512:(hh + 1) * 512], in0=ps[:, :], in1=gt_[:, hh * 512:(hh + 1) * 512], op=ALU.mult),
                             reads=[ps_b, g_b], writes=[tm_b])
                    B.op(B.pool, lambda e: e.tensor_tensor(out=tm[:], in0=tm[:], in1=xt[:], op=ALU.add), reads=[tm_b, xt_b], writes=[tm_b])
                    B.dma(B.sp, self.xcur[r * 128:(r + 1) * 128, :], tm[:], reads=[tm_b])
            B.barrier()

    def phase_moe(self):
        B, Lw = self.B, self.Lw
        last = self.l == 1
        ntiles = NTL if self.need_ctx else 32
        passes = []
        t = 0
        sizes = [10, 8, 8, 8] if ntiles == NTL else [8] * 4
        for sz in sizes:
            passes.append(list(range(t, t + sz)))
            t += sz
        if "moe_tiles" in self.limit:
            passes = [list(self.limit["moe_tiles"])]
        experts = self.limit.get("experts", list(range(NE)))
        wgu = Lw["wgu"].rearrange("(e k p) n -> p e k n", p=128, k=8)
        wdn = Lw["wdn"].rearrange("(e k p) n -> p e k n", p=128, k=8)
        hsrc = self.hT_d.rearrange("(k p) t -> p k t", p=128)
        with contextlib.ExitStack() as st:
            bgu, bgu_b = B.sb(st, [128, NE * 16], F32, "bgu")
            B.dma(B.sp, bgu[:], Lw["bgu"][:, :], writes=[bgu_b])
            bdn, bdn_b = B.sb(st, [NE, D], F32, "bdn")
            B.dma(B.sp, bdn[:], Lw["bdn"][:, :], writes=[bdn_b])
            wring = B.ring(st, 8, [128, 8, 512], BF16, "wx")
            hT, hT_b = B.sb(st, [128, 8, 10 * 128], BF16, "hTm")
            acc, acc_b = B.sb(st, [128, 10, D], F32, "acc")
            actr = B.ring(st, 2, [128, 8, 512], BF16, "actT")
            gr = B.ring(st, 2, [128, 512], F32, "gq")
            sgr = B.ring(st, 2, [128, 512], F32, "sg")
            ur = B.ring(st, 2, [128, 512], F32, "uq")
            xr = B.ring(st, 2, [128, D], F32, "xo")
            rwT, rwT_b = B.sb(st, [NE, 128], F32, "rwT")
            for tiles in passes:
                nt = len(tiles)
                tok0 = tiles[0] * 128
                ntok = nt * 128
                B.dma(B.sp, hT[:, :, 0:ntok], hsrc[:, :, tok0:tok0 + ntok], writes=[hT_b])
                B.op(B.pool, lambda e: e.memset(acc[:, 0:nt, :], 0.0), writes=[acc_b])
                blocks = []
                o = 0
                while o < nt:
                    nb_ = min(4, nt - o)
                    blocks.append((o, nb_))
                    o += nb_
                wts = {}

                def load_w(e_):
                    pcs = []
                    for pi in range(4):
                        wt, wt_b = wring.next()
                        B.dma(B.pool, wt[:], wgu[:, e_, :, pi * 512:(pi + 1) * 512], writes=[wt_b])
                        pcs.append((wt, wt_b))
                    dpc = []
                    for pi in range(2):
                        wt, wt_b = wring.next()
                        B.dma(B.pool, wt[:], wdn[:, e_, :, pi * 512:(pi + 1) * 512], writes=[wt_b])
                        dpc.append((wt, wt_b))
                    wts[e_] = (pcs, dpc)

                def st_gu(item):
                    e_, o, nb_ = item
                    if e_ not in wts:
                        load_w(e_)
                    pcs, dpc = wts[e_]
                    n = nb_ * 128
                    aT, aT_b = actr.next()
                    for fc in range(8):
                        gw, gw_b = pcs[fc // 4]
                        uw, uw_b = pcs[2 + fc // 4]
                        off = (fc % 4) * 128
                        pg, pg_b = self.pring.next()
                        pu, pu_b = self.pring.next()
                        for k in range(8):
                            B.op(B.pe, lambda e: e.matmul(pg[:, 0:n], lhsT=gw[:, k, off:off + 128], rhs=hT[:, k, o * 128:o * 128 + n],
                                                          start=(k == 0), stop=(k == 7)), reads=[gw_b, hT_b], writes=[pg_b])
                        for k in range(8):
                            B.op(B.pe, lambda e: e.matmul(pu[:, 0:n], lhsT=uw[:, k, off:off + 128], rhs=hT[:, k, o * 128:o * 128 + n],
                                                          start=(k == 0), stop=(k == 7)), reads=[uw_b, hT_b], writes=[pu_b])
                        g_, g_b = gr.next()
                        sg, sg_b = sgr.next()
                        u_, u_b = ur.next()
                        bgc = e_ * 16 + fc
                        buc = e_ * 16 + 8 + fc
                        B.op(B.dve, lambda e: e.tensor_scalar(out=g_[:, 0:n], in0=pg[:, 0:n], scalar1=bgu[:, bgc:bgc + 1], scalar2=7.0,
                                                              op0=ALU.add, op1=ALU.min), reads=[pg_b, bgu_b], writes=[g_b])
                        B.op(B.act, lambda e: e.activation(out=sg[:, 0:n], in_=g_[:, 0:n], func=AF.Sigmoid, scale=1.702),
                             reads=[g_b], writes=[sg_b])
                        B.op(B.dve, lambda e: e.tensor_scalar(out=u_[:, 0:n], in0=pu[:, 0:n], scalar1=bgu[:, buc:buc + 1], scalar2=7.0,
                                                              op0=ALU.add, op1=ALU.min), reads=[pu_b, bgu_b], writes=[u_b])
                        B.op(B.dve, lambda e: e.tensor_scalar(out=u_[:, 0:n], in0=u_[:, 0:n], scalar1=-7.0, scalar2=1.0,
                                                              op0=ALU.max, op1=ALU.add), reads=[u_b], writes=[u_b])
                        B.op(B.dve, lambda e: e.tensor_tensor(out=g_[:, 0:n], in0=g_[:, 0:n], in1=sg[:, 0:n], op=ALU.mult),
                             reads=[g_b, sg_b], writes=[g_b])
                        B.op(B.dve, lambda e: e.tensor_tensor(out=aT[:, fc, 0:n], in0=g_[:, 0:n], in1=u_[:, 0:n], op=ALU.mult),
                             reads=[g_b, u_b], writes=[aT_b])
                    return aT, aT_b, dpc

                def st_dn(item, st1):
                    e_, o, nb_ = item
                    aT, aT_b, dpc = st1
                    for sb in range(nb_):
                        ti = o + sb
                        r = tiles[ti]
                        for hh in range(2):
                            dw, dw_b = dpc[hh]
                            py, py_b = self.pring.next()
                            for k in range(8):
                                B.op(B.pe, lambda e: e.matmul(py[:, :], lhsT=aT[:, k, sb * 128:(sb + 1) * 128], rhs=dw[:, k, :],
                                                              start=(k == 0), stop=(k == 7)), reads=[aT_b, dw_b], writes=[py_b])
                            B.op(B.dve, lambda e: e.scalar_tensor_tensor(out=acc[:, ti, hh * 512:(hh + 1) * 512], in0=py[:, :],
                                                                         scalar=self.rw[:, r, e_:e_ + 1], in1=acc[:, ti, hh * 512:(hh + 1) * 512],
                                                                         op0=ALU.mult, op1=ALU.add),
                                 reads=[py_b, self.rw_b, acc_b], writes=[acc_b])

                mitems = [(e_, o, nb_) for e_ in experts for (o, nb_) in blocks]
                prev = None
                for it_ in mitems:
                    cur = st_gu(it_)
                    if prev is not None:
                        st_dn(prev[0], prev[1])
                    prev = (it_, cur)
                st_dn(prev[0], prev[1])
                for ti, r in enumerate(tiles):
                    j = 0 if r < 32 else 1
                    pt, pt_b = self.pring.next()
                    B.op(B.pe, lambda e: e.transpose(out=pt[0:NE, 0:128], in_=self.rw[:, r, :], identity=self.ident[:]),
                         reads=[self.rw_b, self.ident_b], writes=[pt_b])
                    B.op(B.act, lambda e: e.activation(out=rwT[:], in_=pt[0:NE, 0:128], func=AF.Copy), reads=[pt_b], writes=[rwT_b])
                    xt, xt_b = xr.next()
                    B.dma(B.sp, xt[:], self.xcur[r * 128:(r + 1) * 128, :], writes=[xt_b])
                    gt_, g_b = self.gbc[(2, j)]
                    for hh in range(2):
                        py, py_b = self.pring.next()
                        B.op(B.pe, lambda e: e.matmul(py[:, :], lhsT=rwT[:], rhs=bdn[:, hh * 512:(hh + 1) * 512], start=True, stop=True),
                             reads=[rwT_b, bdn_b], writes=[py_b])
                        B.op(B.dve, lambda e: e.tensor_tensor(out=acc[:, ti, hh * 512:(hh + 1) * 512], in0=py[:, :],
                                                              in1=acc[:, ti, hh * 512:(hh + 1) * 512], op=ALU.add),
                             reads=[py_b, acc_b], writes=[acc_b])
                    B.op(B.dve, lambda e: e.tensor_tensor(out=acc[:, ti, :], in0=acc[:, ti, :], in1=gt_[:], op=ALU.mult),
                         reads=[acc_b, g_b], writes=[acc_b])
                    B.op(B.dve, lambda e: e.tensor_tensor(out=xt[:], in0=xt[:], in1=acc[:, ti, :], op=ALU.add),
                         reads=[xt_b, acc_b], writes=[xt_b])
                    if last:
                        B.dma(B.sp, self.y[r * 128:(r + 1) * 128, :], xt[:], reads=[xt_b])
                    else:
                        B.dma(B.sp, self.xcur[r * 128:(r + 1) * 128, :], xt[:], reads=[xt_b])
            B.barrier()
def rope_tabs(s, dim):
    t = np.arange(s * HALF, (s + 1) * HALF, dtype=np.int32)
    row = (t // GRID_W).astype(np.float32)
    col = (t % GRID_W).astype(np.float32)
    nf = dim // 4
    inv = (np.float32(10000.0) ** (-np.arange(nf, dtype=np.float32) / np.float32(nf))).astype(np.float32)
    ang = np.concatenate([row[:, None] * inv, col[:, None] * inv], axis=-1).astype(np.float32)
    cos = np.cos(ang).astype(np.float32)
    sin = np.sin(ang).astype(np.float32)
    f = np.arange(128) % (dim // 2)
    return np.ascontiguousarray(cos[:, f].T), np.ascontiguousarray(sin[:, f].T)


def d_patterns():
    pats = [(10, o) for o in range(-2, 3)]
    pats += [(0, o) for o in range(-2, 4)]
    pats += [(1, o) for o in range(-2, 3)]
    pats += [(30, o) for o in range(-2, 3)]
    pats += [(31, o) for o in range(-3, 3)]
    assert len(pats) == NPD
    return pats


def d_index(s):
    kp = np.arange(128)[:, None]
    qf = np.arange(128)[None, :]
    dr_all, dc_all, mk_all = [], [], []
    for (it, o) in d_patterns():
        g = 32 * s + it
        kt = g + o
        kr = 2 * kt + kp // 64
        kc = kp % 64
        qr = 2 * g + qf // 64
        qc = qf % 64
        r0 = np.clip(qr - 4, 0, 120)
        vrow = (kr >= r0) & (kr <= r0 + 7) & (kr >= 0) & (kr < 128)
        c0 = np.clip(qc - 8, 0, 48)
        vcol = (kc >= c0) & (kc < c0 + 16)
        dr = np.clip(kr - qr + 7, 0, 14)
        dc = np.clip(kc - qc, -15, 15) + 15
        dr_all.append(np.broadcast_to(dr, (128, 128)))
        dc_all.append(np.broadcast_to(dc, (128, 128)))
        mk_all.append(np.broadcast_to(vrow & vcol, (128, 128)))
    return np.stack(dr_all), np.stack(dc_all), np.stack(mk_all)


def consts_host(s):
    c = {}
    c["ident"] = np.eye(128, dtype=np.float32)
    for gs in (64, 32):
        g = np.arange(128) // gs
        c["blk%d" % gs] = (g[:, None] == g[None, :]).astype(np.float32) / np.float32(gs)
        rot = np.zeros((128, 128), np.float32)
        h = gs // 2
        for m in range(128):
            if m % gs < h:
                rot[m + h, m] = -1.0
            else:
                rot[m - h, m] = 1.0
        c["rot%d" % gs] = rot
        co, si = rope_tabs(s, gs)
        c["cos%d" % gs] = co
        c["sin%d" % gs] = si
    kp = np.arange(128)[:, None]
    qf = np.arange(128)[None, :]
    band = [(np.abs(qf - kp - 128 * o) <= 128).astype(np.float32) for o in (-1, 0, 1)]
    zero = np.zeros((128, 128), np.float32)
    cm = np.stack(band + [band[0] if s == 1 else zero, band[2] if s == 0 else zero], axis=1)
    c["cmask"] = cm.reshape(128, 5 * 128)
    _, _, mk = d_index(s)
    c["dmask"] = np.ascontiguousarray(mk.transpose(1, 0, 2).astype(np.float32)).reshape(128, NPD * 128)
    sel = np.zeros((128, 2), np.float32)
    sel[:, 1 - s] = 1.0
    c["sel"] = sel
    return c


def fm(v):
    v = np.asarray(v, np.float32)
    return np.ascontiguousarray(v.reshape(-1, 128).T)


def rep(v):
    v = np.asarray(v, np.float32).reshape(1, -1)
    return np.ascontiguousarray(np.broadcast_to(v, (128, v.shape[1])))


def prep_inputs(inp, prog, n_cores=8):
    shared = {}
    for l in range(2):
        sfx = "_%d" % l
        shared["wada" + sfx] = inp["w_ada"][l]
        shared["bada" + sfx] = fm(inp["b_ada"][l])
        shared["badarow" + sfx] = inp["b_ada"][l][None, :]
        shared["n1g" + sfx] = fm(inp["norm1_g"][l])
        shared["n2g" + sfx] = fm(inp["norm2_g"][l])
        shared["win" + sfx] = inp["w_in"][l]
        shared["bgate" + sfx] = fm(inp["b_gate"][l])
        t64 = lambda v: np.tile(v, 2)
        t32 = lambda v: np.tile(v, 4)
        cols = [t64(inp["a_qn"][l])] * 2 + [t32(inp["b_qn"][l])] * 2 + [t64(inp["c_qn"][l])] * 2 + [t64(inp["d_qn"][l])] * 2
        cols += [t64(inp["a_kn"][l])] + [t32(inp["b_kn"][l])] * 2 + [t64(inp["c_kn"][l])] + [t64(inp["d_kn"][l])] * 2
        shared["qkg" + sfx] = np.ascontiguousarray(np.stack(cols, axis=1).astype(np.float32))
        shared["lamv" + sfx] = rep(np.concatenate([inp["lam_q1"][l], inp["lam_k1"][l], inp["lam_q2"][l], inp["lam_k2"][l]]))
        shared["subg" + sfx] = rep(inp["subln_g"][l])
        shared["sinkb" + sfx] = rep(inp["sink"][l])
        shared["wbr" + sfx] = inp["w_branch"][l].reshape(D, D)
        shared["wout" + sfx] = inp["w_out"][l]
        shared["rtw" + sfx] = inp["router_w"][l]
        shared["rtb" + sfx] = rep(inp["router_b"][l])
        shared["wgu" + sfx] = inp["w_gate_up"][l].reshape(NE * D, 2 * D)
        shared["bgu" + sfx] = np.ascontiguousarray(inp["b_gate_up"][l].reshape(NE, 16, 128).transpose(2, 0, 1)).reshape(128, NE * 16)
        shared["wdn" + sfx] = inp["w_down"][l].reshape(NE * D, D)
        shared["bdn" + sfx] = inp["b_down"][l]
    shared = {k: np.ascontiguousarray(v, dtype=np.float32) for k, v in shared.items() if k in prog.ins}
    half = []
    for s in range(2):
        h = consts_host(s)
        dr, dc, _ = d_index(s)
        for l in range(2):
            tab = inp["rpb"][l][:, dr, dc]
            h["dtab_%d" % l] = np.ascontiguousarray(tab.transpose(2, 1, 0, 3)).reshape(128, NPD * 4 * 128)
        half.append({k: np.ascontiguousarray(v, dtype=np.float32) for k, v in h.items() if k in prog.ins})
    maps = []
    for c in range(n_cores):
        b, s = c // 2, c % 2
        m = dict(shared)
        m.update(half[s])
        m["xin"] = np.ascontiguousarray(np.concatenate([inp["x"][b, s * HALF:(s + 1) * HALF], inp["ctx"][b]], axis=0))
        m["cvec"] = np.ascontiguousarray(np.stack([fm(inp["c"][b]), fm(inp["c_ctx"])], axis=-1).reshape(128, 16))
        maps.append(m)
    return maps


def kernel(**inputs):
    inp = {k: np.asarray(v) for k, v in inputs.items()}
    prog = Prog()
    nc = prog.build()
    maps = prep_inputs(inp, prog)
    res = run_bass_kernel_spmd(nc, maps, core_ids=list(range(8)))
    out = np.zeros((4, SEQ, D), np.float32)
    for c in range(8):
        b, s = c // 2, c % 2
        out[b, s * HALF:(s + 1) * HALF] = res.results[c]["y"]
    return out
```
